# Optimizing a Trainium2 kernel written in Bass

```python
import math
import jax, jax.numpy as jnp
from jax import lax
import numpy as np

D_MODEL = 1024
BATCH = 32
SEQ = 2048
DEPTH = 1

N_HEADS = 8
HEAD_DIM = 64
N_KV_HEADS = 2
IDX_HEADS = 8
IDX_DIM = 64
TOPK_MAX = 256
Q_BLOCK = 64
N_BUCKETS = 32
MAX_DISTANCE = 128
D_RNN = D_MODEL
N_RNN_BLOCKS = 8
RNN_BLOCK = D_RNN // N_RNN_BLOCKS
RNN_CONV = 4
LRU_C = 8.0
D_FF = 2816
FFN_CONV = 3
EPS = 1e-6

ATTN_WIDTH = N_HEADS * HEAD_DIM
KV_WIDTH = N_KV_HEADS * HEAD_DIM
SPLITS = (ATTN_WIDTH, KV_WIDTH, KV_WIDTH, IDX_HEADS * IDX_DIM, IDX_DIM, IDX_HEADS, D_RNN, D_RNN, D_MODEL, D_MODEL)
N_IN = 5448

kernel_name = "hybrid_dsa_rglru_convffn_block"


def rms_norm(x, g):
    x32 = x.astype(jnp.float32)
    y = x32 * lax.rsqrt(jnp.mean(x32 * x32, axis=-1, keepdims=True) + EPS)
    return (y * g.astype(jnp.float32)).astype(x.dtype)


def causal_dwconv(x, w, b):
    width = w.shape[0]
    seq = x.shape[1]
    xp = jnp.pad(x, ((0, 0), (width - 1, 0), (0, 0)))
    y = xp[:, 0:seq] * w[0]
    for k in range(1, width):
        y = y + xp[:, k:k + seq] * w[k]
    return y + b


def t5_bucket(rel):
    max_exact = N_BUCKETS // 2
    n = jnp.maximum(rel, 0)
    nf = jnp.maximum(n, 1).astype(jnp.float32)
    large = max_exact + (jnp.log(nf / max_exact) / math.log(MAX_DISTANCE / max_exact)
                         * (N_BUCKETS - max_exact)).astype(jnp.int32)
    large = jnp.minimum(large, N_BUCKETS - 1)
    return jnp.where(n < max_exact, n, large)


def dsa_attention(q, k, v, q_idx, k_idx, w_idx, rel_bias):
    B, S = q.shape[0], q.shape[1]
    n_sel = min(TOPK_MAX, S // 4)
    nb = S // Q_BLOCK
    G = N_HEADS // N_KV_HEADS
    pos = jnp.arange(S, dtype=jnp.int32)
    gather = jax.vmap(lambda t_b, i_b: t_b[i_b])

    def to_blocks(t):
        return jnp.moveaxis(t.reshape((B, nb, Q_BLOCK) + t.shape[2:]), 1, 0)

    def block(args):
        qb, qib, wb, tb = args
        dots = jnp.einsum('bqhd,bsd->bqhs', qib, k_idx).astype(jnp.float32) * (IDX_DIM ** -0.5)
        score = jnp.einsum('bqh,bqhs->bqs', wb.astype(jnp.float32) * (IDX_HEADS ** -0.5), jax.nn.relu(dots))
        causal = pos[None, :] <= tb[:, None]
        score = jnp.where(causal[None], score, -jnp.inf)
        _, sel = lax.top_k(score, n_sel)
        valid = sel <= tb[None, :, None]
        ks = gather(k, sel)
        vs = gather(v, sel)
        qg = qb.reshape(B, Q_BLOCK, N_KV_HEADS, G, HEAD_DIM)
        logits = jnp.einsum('bqhgd,bqnhd->bqhgn', qg, ks).astype(jnp.float32) * (HEAD_DIM ** -0.5)
        bucket = t5_bucket(tb[None, :, None] - sel)
        bias = rel_bias[bucket].astype(jnp.float32)
        bias = bias.reshape(B, Q_BLOCK, n_sel, N_KV_HEADS, G).transpose(0, 1, 3, 4, 2)
        logits = jnp.where(valid[:, :, None, None, :], logits + bias, -jnp.inf)
        p = jax.nn.softmax(logits, axis=-1).astype(vs.dtype)
        o = jnp.einsum('bqhgn,bqnhd->bqhgd', p, vs)
        return o.reshape(B, Q_BLOCK, N_HEADS * HEAD_DIM)

    out = lax.map(block, (to_blocks(q), to_blocks(q_idx), to_blocks(w_idx), pos.reshape(nb, Q_BLOCK)))
    return jnp.moveaxis(out, 0, 1).reshape(B, S, N_HEADS * HEAD_DIM)


def rg_lru(x, w_rg, b_rg, w_ig, b_ig, lam):
    B, S = x.shape[0], x.shape[1]
    xb = x.reshape(B, S, N_RNN_BLOCKS, RNN_BLOCK)
    r = jax.nn.sigmoid(jnp.einsum('bsnc,ncd->bsnd', xb, w_rg).reshape(B, S, D_RNN) + b_rg)
    i = jax.nn.sigmoid(jnp.einsum('bsnc,ncd->bsnd', xb, w_ig).reshape(B, S, D_RNN) + b_ig)
    log_a = -LRU_C * r.astype(jnp.float32) * jax.nn.softplus(-lam.astype(jnp.float32))
    a = jnp.exp(log_a)
    mult = jnp.sqrt(-jnp.expm1(2.0 * log_a))
    u = x.astype(jnp.float32) * i.astype(jnp.float32) * mult

    def combine(left, right):
        a1, b1 = left
        a2, b2 = right
        return a1 * a2, a2 * b1 + b2

    _, h = lax.associative_scan(combine, (a, u), axis=1)
    return h.astype(x.dtype)


def setup_inputs(seed: int = 0) -> dict:
    key = jax.random.key(seed)
    ks = jax.random.split(key, 32)
    f32 = jnp.float32

    def nrm(k, shape, scale):
        return jax.random.normal(k, shape, f32) * scale

    L = DEPTH
    a0 = jax.random.uniform(ks[13], (L, D_RNN), f32, 0.9, 0.999)
    p = a0 ** (1.0 / LRU_C)
    lam = jnp.log(p) - jnp.log1p(-p)
    return {
        "x": nrm(ks[0], (BATCH, SEQ, D_MODEL), 1.0),
        "c": nrm(ks[1], (BATCH, D_MODEL), 1.0),
        "w_ada": nrm(ks[2], (L, D_MODEL, 6 * D_MODEL), D_MODEL ** -0.5),
        "b_ada": nrm(ks[3], (L, 6 * D_MODEL), 0.02),
        "g_mix": 1.0 + nrm(ks[4], (L, D_MODEL), 0.02),
        "w_in": nrm(ks[5], (L, D_MODEL, N_IN), D_MODEL ** -0.5),
        "b_in": nrm(ks[6], (L, N_IN), 0.01),
        "rel_bias": nrm(ks[7], (N_BUCKETS, N_HEADS), 0.5),
        "conv_rnn_w": nrm(ks[8], (L, RNN_CONV, D_RNN), RNN_CONV ** -0.5),
        "conv_rnn_b": nrm(ks[9], (L, D_RNN), 0.01),
        "w_rg": nrm(ks[10], (L, N_RNN_BLOCKS, RNN_BLOCK, RNN_BLOCK), RNN_BLOCK ** -0.5),
        "b_rg": nrm(ks[11], (L, D_RNN), 0.01),
        "w_ig": nrm(ks[12], (L, N_RNN_BLOCKS, RNN_BLOCK, RNN_BLOCK), RNN_BLOCK ** -0.5),
        "b_ig": nrm(ks[14], (L, D_RNN), 0.01),
        "lru_lambda": lam,
        "w_o_attn": nrm(ks[15], (L, ATTN_WIDTH, D_MODEL), ATTN_WIDTH ** -0.5),
        "w_o_rnn": nrm(ks[16], (L, D_RNN, D_MODEL), D_RNN ** -0.5),
        "w_out": nrm(ks[17], (L, D_MODEL, D_MODEL), D_MODEL ** -0.5),
        "g_ffn": 1.0 + nrm(ks[18], (L, D_MODEL), 0.02),
        "w_up": nrm(ks[19], (L, D_MODEL, 2 * D_FF), D_MODEL ** -0.5),
        "conv_ffn_w": nrm(ks[20], (L, FFN_CONV, 2 * D_FF), FFN_CONV ** -0.5),
        "conv_ffn_b": nrm(ks[21], (L, 2 * D_FF), 0.01),
        "w_down": nrm(ks[22], (L, D_FF, D_MODEL), D_FF ** -0.5),
        "g_final": 1.0 + nrm(ks[23], (D_MODEL,), 0.02),
    }


def reference(x, c, w_ada, b_ada, g_mix, w_in, b_in, rel_bias, conv_rnn_w, conv_rnn_b,
              w_rg, b_rg, w_ig, b_ig, lru_lambda, w_o_attn, w_o_rnn, w_out, g_ffn,
              w_up, conv_ffn_w, conv_ffn_b, w_down, g_final):
    B, S = x.shape[0], x.shape[1]
    cuts = []
    acc = 0
    for width in SPLITS[:-1]:
        acc += width
        cuts.append(acc)
    c_act = jax.nn.silu(c)
    h = x
    for l in range(DEPTH):
        mod = jnp.dot(c_act, w_ada[l]) + b_ada[l]
        sh1, sc1, ga1, sh2, sc2, ga2 = [m[:, None, :] for m in jnp.split(mod, 6, axis=-1)]

        xn = rms_norm(h, g_mix[l]) * (1.0 + sc1) + sh1
        proj = jnp.dot(xn, w_in[l]) + b_in[l]
        q, k, v, qi, ki, wi, xr, yr, gate_a, gate_b = jnp.split(proj, cuts, axis=-1)

        attn = dsa_attention(
            q.reshape(B, S, N_HEADS, HEAD_DIM),
            k.reshape(B, S, N_KV_HEADS, HEAD_DIM),
            v.reshape(B, S, N_KV_HEADS, HEAD_DIM),
            qi.reshape(B, S, IDX_HEADS, IDX_DIM), ki, wi, rel_bias)

        xr = causal_dwconv(xr, conv_rnn_w[l], conv_rnn_b[l])
        rnn = rg_lru(xr, w_rg[l], b_rg[l], w_ig[l], b_ig[l], lru_lambda[l]) * jax.nn.gelu(yr)

        merged = (jax.nn.sigmoid(gate_a) * jnp.dot(attn, w_o_attn[l])
                  + jax.nn.sigmoid(gate_b) * jnp.dot(rnn, w_o_rnn[l]))
        h = h + ga1 * jnp.dot(merged, w_out[l])

        xn = rms_norm(h, g_ffn[l]) * (1.0 + sc2) + sh2
        up = causal_dwconv(jnp.dot(xn, w_up[l]), conv_ffn_w[l], conv_ffn_b[l])
        val, gte = jnp.split(up, 2, axis=-1)
        h = h + ga2 * jnp.dot(jax.nn.silu(gte) * val, w_down[l])
    return rms_norm(h, g_final)
```

```python
import math
from contextlib import ExitStack
import numpy as np
import concourse.bass as bass
import concourse.mybir as mybir
from concourse.bass_utils import run_bass_kernel_spmd

F32 = mybir.dt.float32
BF16 = mybir.dt.bfloat16
U8 = mybir.dt.uint8
AF = mybir.ActivationFunctionType
ALU = mybir.AluOpType
AX = mybir.AxisListType

NCORES = 8
S = 2048
D = 1024
NH = 8
DFF = 2816
NJ = 22
TB = 512
NBLK = S // TB
N_IN = 5448
EPS = 1e-6
NIT = 16
NSEL = 256
FF_GROUPS = [list(range(0, 6)), list(range(6, 12)), list(range(12, 17)), list(range(17, 22))]
WIN_GROUPS = ([("q%d" % i, 128 * i) for i in range(4)] + [("k", 512), ("v", 640)]
              + [("qi%d" % i, 768 + 128 * i) for i in range(4)] + [("kiwi", 1280)]
              + [("xr%d" % i, 1352 + 128 * i) for i in range(8)]
              + [("yr%d" % i, 2376 + 128 * i) for i in range(8)]
              + [("ga%d" % i, 3400 + 128 * i) for i in range(8)]
              + [("gb%d" % i, 4424 + 128 * i) for i in range(8)])
WIN_IDX = {n: i for i, (n, _) in enumerate(WIN_GROUPS)}
NWG = len(WIN_GROUPS)


class Trk:
    __slots__ = ("w", "r", "dsem", "dcnt", "name")

    def __init__(self, name=""):
        self.w = []
        self.r = []
        self.dsem = None
        self.dcnt = 0
        self.name = name

    def events(self):
        return list(self.w) + list(self.r)


ENGS = ("pe", "act", "dve", "pool", "sp")


class Rec:
    def __init__(self, n_dma_sems):
        self.streams = {e: [] for e in ENGS}
        self.count = {e: 0 for e in ENGS}
        self.waited = {e: {} for e in ENGS}
        self.n_dma_sems = n_dma_sems
        self.next_dsem = 0
        self.pending = {e: False for e in ENGS}

    def _waits(self, eng, deps):
        st = self.streams[eng]
        wd = self.waited[eng]
        for (sk, v) in deps:
            if eng == "pe" and sk == "pe":
                continue
            if wd.get(sk, 0) < v:
                st.append(("w", sk, v))
                wd[sk] = v

    @staticmethod
    def _deps(reads, writes):
        deps = []
        for t in reads:
            deps += t.w
        for t in writes:
            deps += t.w
            deps += t.r
        return deps

    @staticmethod
    def _commit(ev, reads, writes):
        for t in writes:
            t.w = [ev]
            t.r = []
        for t in reads:
            if t in writes:
                continue
            t.r = [e for e in t.r if e[0] != ev[0]] + [ev]

    def op(self, eng, fn, reads=(), writes=(), signal=True):
        self._waits(eng, self._deps(reads, writes))
        if signal:
            self.count[eng] += 1
            ev = (eng, self.count[eng])
            self.streams[eng].append(("i", fn))
            self.pending[eng] = False
        else:
            ev = (eng, self.count[eng] + 1)
            self.streams[eng].append(("n", fn))
            self.pending[eng] = True
        self._commit(ev, reads, writes)

    def dma(self, q, fn, reads=(), writes=(), semtrk=None):
        self._waits(q, self._deps(reads, writes))
        t = semtrk if semtrk is not None else (writes[0] if writes else reads[0])
        if t.dsem is None:
            assert self.next_dsem < self.n_dma_sems, "out of dma sems"
            t.dsem = "d%d" % self.next_dsem
            self.next_dsem += 1
        t.dcnt += 16
        ev = (t.dsem, t.dcnt)
        self.streams[q].append(("d", fn, t.dsem))
        self._commit(ev, reads, writes)

    def wait_all(self, eng, trks):
        deps = []
        for t in trks:
            deps += t.events()
        self._waits(eng, deps)

    @staticmethod
    def fence(old, new):
        evs = {}
        for t in old:
            for (sk, v) in t.events():
                if evs.get(sk, 0) < v:
                    evs[sk] = v
        for t in new:
            cur = {}
            for (sk, v) in t.events() + list(evs.items()):
                if cur.get(sk, 0) < v:
                    cur[sk] = v
            t.r = list(cur.items())


def _t5_bucket_np(n):
    n = np.maximum(n, 0)
    nf = np.maximum(n, 1).astype(np.float32)
    large = 16 + (np.log(nf / np.float32(16)) / np.float32(math.log(128 / 16)) * np.float32(16)).astype(np.int32)
    large = np.minimum(large, 31)
    return np.where(n < 16, n, large)


class VecPack:
    def __init__(self):
        self.cols = []
        self.off = {}
        self.n = 0

    def add(self, name, arr):
        arr = np.asarray(arr, np.float32)
        assert arr.shape[0] == 128
        self.off[name] = self.n
        self.cols.append(arr)
        self.n += arr.shape[1]

    def pack(self):
        return np.ascontiguousarray(np.concatenate(self.cols, axis=1))


def _fm(v, nch):
    v = np.asarray(v, np.float32).reshape(nch, 128)
    return v.T


def _pad_rows64(a):
    out = np.zeros((128, a.shape[1]), np.float32)
    out[:64] = a
    return out


def _layout_shared(inp):
    w_in = inp["w_in"][0]
    b_in = inp["b_in"][0]
    sh = {}
    wt = np.zeros((NWG, 128, 8, 128), np.float32)
    for gi, (name, c0) in enumerate(WIN_GROUPS):
        wdt = 72 if name == "kiwi" else 128
        blk = w_in[:, c0:c0 + wdt].reshape(8, 128, wdt)
        wt[gi, :, :, :wdt] = blk.transpose(1, 0, 2)
    sh["win_t"] = wt.reshape(NWG * 128, 1024)
    w_up = inp["w_up"][0]
    wu = np.zeros((NJ, 128, 8, 256), np.float32)
    for j in range(NJ):
        wu[j, :, :, 0:128] = w_up[:, 128 * j:128 * j + 128].reshape(8, 128, 128).transpose(1, 0, 2)
        wu[j, :, :, 128:256] = w_up[:, DFF + 128 * j:DFF + 128 * j + 128].reshape(8, 128, 128).transpose(1, 0, 2)
    sh["wup_t"] = wu.reshape(NJ * 128, 2048)
    sh["w_down"] = np.ascontiguousarray(inp["w_down"][0])
    woa = inp["w_o_attn"][0].reshape(8, 64, 8, 128)
    sh["woattn_t"] = np.ascontiguousarray(woa.transpose(2, 1, 0, 3)).reshape(8 * 64, 1024)
    wor = inp["w_o_rnn"][0].reshape(8, 128, 8, 128)
    sh["wornn_t"] = np.ascontiguousarray(wor.transpose(2, 1, 0, 3)).reshape(8 * 128, 1024)
    sh["w_out"] = np.ascontiguousarray(inp["w_out"][0])
    wg = np.concatenate([inp["w_rg"][0].transpose(1, 0, 2).reshape(128, 1024),
                         inp["w_ig"][0].transpose(1, 0, 2).reshape(128, 1024)], axis=1)
    sh["wgate_t"] = np.ascontiguousarray(wg)
    sh["w_ada"] = np.ascontiguousarray(inp["w_ada"][0])

    pv = VecPack()
    pv.add("gmix", _fm(inp["g_mix"][0], 8))
    pv.add("gffn", _fm(inp["g_ffn"][0], 8))
    pv.add("bq", _pad_rows64(b_in[0:512].reshape(8, 64).T))
    pv.add("bk", _pad_rows64(b_in[512:640].reshape(2, 64).T))
    pv.add("bqi", _pad_rows64(b_in[768:1280].reshape(8, 64).T))
    pv.add("bki", _pad_rows64(b_in[1280:1344].reshape(1, 64).T))
    pv.add("bxr", _fm(b_in[1352:2376], 8))
    pv.add("byr", _fm(b_in[2376:3400], 8))
    pv.add("bga", _fm(b_in[3400:4424], 8))
    pv.add("bgb", _fm(b_in[4424:5448], 8))
    crw = inp["conv_rnn_w"][0]
    pv.add("crw", crw.reshape(4, 8, 128).transpose(2, 1, 0).reshape(128, 32))
    pv.add("crb", _fm(inp["conv_rnn_b"][0], 8))
    pv.add("brg", _fm(inp["b_rg"][0], 8))
    pv.add("big", _fm(inp["b_ig"][0], 8))
    pv.add("lam", _fm(inp["lru_lambda"][0], 8))
    cfw = inp["conv_ffn_w"][0]
    pv.add("cfw", cfw.reshape(3, 44, 128).transpose(2, 1, 0).reshape(128, 132))
    pv.add("cfb", _fm(inp["conv_ffn_b"][0], 44))
    b_ada = inp["b_ada"][0]
    for nm, blk in (("sh1", 0), ("sc1", 1), ("sh2", 3), ("sc2", 4)):
        pv.add("ada_" + nm, _fm(b_ada[blk * 1024:(blk + 1) * 1024], 8))
    sh["pvec"] = pv.pack()

    bc = VecPack()
    rep = lambda v: np.broadcast_to(np.asarray(v, np.float32)[None, :], (128, len(v)))
    bc.add("bv", rep(b_in[640:768]))
    bc.add("bwi", rep(b_in[1344:1352]))
    bc.add("b31", rep(inp["rel_bias"][31, :]))
    bc.add("gfin", rep(inp["g_final"]))
    sh["bcv"] = bc.pack()
    sh["bcada"] = np.ascontiguousarray(np.concatenate([rep(b_ada[2048:3072]), rep(b_ada[5120:6144])], axis=1))

    rb = inp["rel_bias"]
    sl = np.arange(128)[:, None]
    tl = np.arange(128)[None, :]
    d0 = tl - sl
    d1 = 128 + tl - sl
    T = np.zeros((128, 8, 256), np.float32)
    b0 = rb[_t5_bucket_np(d0)]
    b1 = rb[_t5_bucket_np(d1)]
    T[:, :, 0:128] = np.where((d0 >= 0)[:, :, None], b0, np.float32(-30000.0)).transpose(0, 2, 1)
    T[:, :, 128:256] = b1.transpose(0, 2, 1)
    negm = np.where(np.arange(128)[None, :] <= np.arange(128)[:, None], 0.0, -1e30).astype(np.float32)
    ident = np.eye(128, dtype=np.float32)
    sh["tbl"] = np.ascontiguousarray(np.concatenate([T.reshape(128, 2048), negm, ident], axis=1))
    return sh, pv.off, bc.off


PV_OFF = None
BC_OFF = None


def _offsets():
    global PV_OFF, BC_OFF
    if PV_OFF is None:
        z = lambda *s: np.zeros(s, np.float32)
        fake = {"w_in": z(1, 1024, N_IN), "b_in": z(1, N_IN), "w_up": z(1, 1024, 2 * DFF), "w_down": z(1, DFF, 1024),
                "w_o_attn": z(1, 512, 1024), "w_o_rnn": z(1, 1024, 1024), "w_out": z(1, 1024, 1024),
                "w_rg": z(1, 8, 128, 128), "w_ig": z(1, 8, 128, 128), "w_ada": z(1, 1024, 6144),
                "g_mix": z(1, 1024), "g_ffn": z(1, 1024), "conv_rnn_w": z(1, 4, 1024), "conv_rnn_b": z(1, 1024),
                "b_rg": z(1, 1024), "b_ig": z(1, 1024), "lru_lambda": z(1, 1024), "conv_ffn_w": z(1, 3, 2 * DFF),
                "conv_ffn_b": z(1, 2 * DFF), "b_ada": z(1, 6144), "rel_bias": z(32, 8), "g_final": z(1024)}
        _, PV_OFF, BC_OFF = _layout_shared(fake)
    return PV_OFF, BC_OFF


def build_nc(nseq=4, nblk=NBLK, dbg=False):
    pvo, bco = _offsets()
    NPV = max(pvo.values()) + 64
    nc = bass.Bass("TRN2", target_bir_lowering=False)
    R = Rec(n_dma_sems=56)

    def din(name, shape, dt=F32):
        return nc.dram_tensor(name, list(shape), dt, kind="ExternalInput")

    x_d = din("x", [nseq, S, D])
    cT_d = din("cT", [128, 8 * nseq])
    wada_d = din("w_ada", [1024, 6144])
    pvec_d = din("pvec", [128, _PVN()])
    bcv_d = din("bcv", [128, 128 + 8 + 8 + 1024])
    bcada_d = din("bcada", [128, 2048])
    tbl_d = din("tbl", [128, 2048 + 256])
    win_d = din("win_t", [NWG * 128, 1024])
    wup_d = din("wup_t", [NJ * 128, 2048])
    wdown_d = din("w_down", [DFF, 1024])
    woa_d = din("woattn_t", [512, 1024])
    wor_d = din("wornn_t", [1024, 1024])
    wout_d = din("w_out", [1024, 1024])
    wgate_d = din("wgate_t", [128, 2048])
    out_d = nc.dram_tensor("out", [nseq, S, D], F32, kind="ExternalOutput")
    wb_in = nc.dram_tensor("wb_in", [NWG * 128, 1024], BF16)
    wb_up = nc.dram_tensor("wb_up", [NJ * 128, 2048], BF16)
    wb_down = nc.dram_tensor("wb_down", [DFF, 1024], BF16)
    wb_oa = nc.dram_tensor("wb_oa", [512, 1024], BF16)
    wb_or = nc.dram_tensor("wb_or", [1024, 1024], BF16)
    wb_out = nc.dram_tensor("wb_out", [1024, 1024], BF16)
    gabc_d = nc.dram_tensor("gabc_d", [nseq * 128, 2048], F32)
    t_wb = {k: Trk("wb_" + k) for k in ("in", "up", "down", "oa", "or", "out")}
    t_gabc_d = [Trk("gabc_d%d" % b) for b in range(nseq)]

    es = ExitStack()
    with es:
        es.enter_context(nc.allow_low_precision("bf16 matmul operands, fp32 accumulation (problem tolerance)"))

        def sb(name, shape, dt):
            return es.enter_context(nc.sbuf_tensor("sb_" + name, list(shape), dt))

        sems = {}
        for e in ENGS:
            sems[e] = es.enter_context(nc.semaphore("s_" + e))
        for i in range(R.n_dma_sems):
            sems["d%d" % i] = es.enter_context(nc.semaphore("s_d%d" % i))

        pvec = sb("pvec", [128, _PVN()], F32); t_pvec = Trk()
        bcv = sb("bcv", [128, 128 + 8 + 8 + 1024], F32); t_bcv = Trk()
        tbl = sb("tbl", [128, 2048 + 256], F32); t_tbl = Trk()
        identb = sb("identb", [128, 128], BF16); t_identb = Trk()
        ones64 = sb("ones64", [64, 128], BF16); t_ones64 = Trk()
        onesf = sb("onesf", [128, 64], F32); t_onesf = Trk()
        wgate = sb("wgate", [128, 2048], BF16); t_wgate = Trk()
        modT = sb("modT", [128, 4 * 8 * nseq], F32); t_modT = Trk()
        gs = sb("gs", [128, 2 * 8 * nseq], F32); t_gs = Trk()
        cneg = sb("cneg", [128, 16], F32); t_cneg = Trk()
        bq8 = sb("bq8", [64, 8], F32); t_bq8 = Trk()
        gabc = sb("gabc", [128, 2048], F32); t_gabc = Trk()
        kT = sb("kT", [64, 2 * S], BF16); t_kT = Trk()
        vaug = sb("vaug", [128, 16 * 2 * 65], BF16); t_vaug = Trk()
        kiT = sb("kiT", [64, S], BF16); t_kiT = Trk()
        halo_xr = sb("halo_xr", [128, 8 * 3], F32); t_hxr = [Trk() for _ in range(8)]
        halo_up = sb("halo_up", [128, 44 * 2], F32); t_hup = [Trk() for _ in range(44)]
        hl = sb("hl", [128, 8], F32); t_hl = [Trk() for _ in range(8)]
        kmaxsq = sb("kmaxsq", [128, 2], F32); t_kmax = Trk()
        xh = [sb("xh%d" % i, [128, D], F32) for i in range(4)]; t_xh = [Trk() for _ in range(4)]
        mergedT = sb("mergedT", [128, 8 * TB], BF16); t_merged = [Trk() for _ in range(8)]
        small = sb("small", [128, 64], F32)
        t_small = {}

        def smc(name, c0, w=1):
            if name not in t_small:
                t_small[name] = (Trk(name), c0, w)
            t, c, ww = t_small[name]
            return small[:, c:c + ww], t

        ARENA_BYTES = 130 * 1024
        arena = sb("arena", [128, ARENA_BYTES // 2], BF16)
        ar_f32 = arena.bitcast(F32)
        ar_u8 = arena.bitcast(U8)

        class Lay:
            def __init__(self):
                self.off = 0
                self.trks = []

            def alloc(self, nbytes, dt, parts=128):
                o = self.off
                self.off += (nbytes + 63) // 64 * 64
                assert self.off <= ARENA_BYTES, ("arena overflow", self.off)
                h = {BF16: arena, F32: ar_f32, U8: ar_u8}[dt]
                esz = {BF16: 2, F32: 4, U8: 1}[dt]
                return h[0:parts, o // esz:(o + nbytes) // esz]

            def trk(self, name=""):
                t = Trk(name)
                self.trks.append(t)
                return t

        LM = Lay()
        xnT = LM.alloc(8 * TB * 2, BF16); t_xnT = [LM.trk() for _ in range(4)]
        xs = [LM.alloc(D * 2, BF16) for _ in range(2)]; t_xs = [LM.trk() for _ in range(2)]
        junkA = LM.alloc(D * 2, BF16); t_junkA = LM.trk()
        qT = LM.alloc(8 * TB * 2, BF16, 64); t_q = [LM.trk() for _ in range(8)]
        qiT = LM.alloc(8 * TB * 2, BF16, 64); t_qi = [LM.trk() for _ in range(8)]
        sqt = [LM.alloc(TB * 2, BF16, 64) for _ in range(2)]; t_sqt = [LM.trk() for _ in range(2)]
        wis = LM.alloc(4 * 8 * 4, F32); t_wis = [LM.trk() for _ in range(4)]
        sc = LM.alloc(S * 4, F32); t_sc = LM.trk()
        junkD = LM.alloc(S, U8); t_junkD = LM.trk()
        rl = [LM.alloc(TB * 4, F32) for _ in range(2)]; t_rl = [LM.trk() for _ in range(2)]
        mask = LM.alloc(S * 2, BF16); t_mask = LM.trk()
        maskT = LM.alloc(16 * TB * 2, BF16); t_maskT = [LM.trk() for _ in range(4)]
        NPT = 4
        PT = [LM.alloc(TB * 2, BF16) for _ in range(NPT)]; t_PT = [LM.trk() for _ in range(NPT)]
        nr = [LM.alloc(256 * 4, F32) for _ in range(3)]; t_nr = [LM.trk() for _ in range(3)]
        osb = [LM.alloc(TB * 4, F32, 65) for _ in range(2)]; t_osb = [LM.trk() for _ in range(2)]
        attnT = LM.alloc(8 * TB * 2, BF16, 64); t_attn = [LM.trk() for _ in range(8)]
        xrb = [LM.alloc(516 * 4, F32) for _ in range(2)]; t_xrb = [LM.trk() for _ in range(2)]
        xc = [LM.alloc(TB * 4, F32) for _ in range(2)]; t_xc = [LM.trk() for _ in range(2)]
        xcb = [LM.alloc(TB * 2, BF16) for _ in range(2)]; t_xcb = [LM.trk() for _ in range(2)]
        rnn_tmp_off = LM.off
        rr = LM.alloc(TB * 4, F32); t_rr = LM.trk()
        ii = LM.alloc(TB * 4, F32); t_ii = LM.trk()
        aa = LM.alloc(TB * 4, F32); t_aa = LM.trk()
        a2 = LM.alloc(TB * 4, F32); t_a2 = LM.trk()
        rnn_alias_trks = [t_rr, t_ii, t_aa, t_a2]
        uu = LM.alloc(TB * 4, F32); t_uu = LM.trk()
        hh = LM.alloc(TB * 4, F32); t_hh = LM.trk()
        yy = [LM.alloc(TB * 4, F32) for _ in range(2)]; t_yy = [LM.trk() for _ in range(2)]
        y2 = LM.alloc(TB * 4, F32); t_y2 = LM.trk()
        rnnT = LM.alloc(8 * TB * 2, BF16); t_rnn = [LM.trk() for _ in range(8)]
        _save = LM.off
        LM.off = rnn_tmp_off
        sa = LM.alloc(TB * 4, F32); t_sa = LM.trk()
        sbg = LM.alloc(TB * 4, F32); t_sbg = LM.trk()
        t1 = LM.alloc(TB * 4, F32); t_t1 = LM.trk()
        t2 = LM.alloc(TB * 4, F32); t_t2 = LM.trk()
        merge_alias_trks = [t_sa, t_sbg, t_t1, t_t2]
        LM.off = _save
        NST = 4
        stg = [LM.alloc(8 * 128 * 2, BF16) for _ in range(NST)]; t_stg = [LM.trk() for _ in range(NST)]
        stg_i = [0]

        LF = Lay()
        hnT = LF.alloc(8 * TB * 2, BF16); t_hnT = [LF.trk() for _ in range(4)]
        xs2 = [LF.alloc(D * 2, BF16) for _ in range(2)]; t_xs2 = [LF.trk() for _ in range(2)]
        junkF = LF.alloc(D * 2, BF16); t_junkF = LF.trk()
        dtmp = LF.alloc(D * 4, F32); t_dtmp = LF.trk()
        wout_s = LF.alloc(8 * 1024 * 2, BF16); t_wout = LF.trk()
        ubv = [LF.alloc(516 * 4, F32) for _ in range(2)]; t_ubv = [LF.trk() for _ in range(2)]
        ubg = [LF.alloc(516 * 4, F32) for _ in range(2)]; t_ubg = [LF.trk() for _ in range(2)]
        cv = [LF.alloc(TB * 4, F32) for _ in range(2)]; t_cv = [LF.trk() for _ in range(2)]
        cg = [LF.alloc(TB * 4, F32) for _ in range(2)]; t_cg = [LF.trk() for _ in range(2)]
        sgb = [LF.alloc(TB * 4, F32) for _ in range(2)]; t_sg = [LF.trk() for _ in range(2)]
        actb = [LF.alloc(6 * TB * 2, BF16) for _ in range(2)]; t_act = [LF.trk() for _ in range(2)]
        wd_s = [LF.alloc(6 * 1024 * 2, BF16) for _ in range(2)]; t_wd = [LF.trk() for _ in range(2)]
        NSU = 3
        stu = [LF.alloc(8 * 256 * 2, BF16) for _ in range(NSU)]; t_stu = [LF.trk() for _ in range(NSU)]
        stu_i = [0]
        otile = [LF.alloc(D * 4, F32) for _ in range(2)]; t_ot = [LF.trk() for _ in range(2)]

        LS = Lay()
        wst = LS.alloc(8 * 1024 * 2, BF16); t_wst = LS.trk()
        cTs = LS.alloc(8 * nseq * 4, F32); t_cTs = LS.trk()
        cact = LS.alloc(8 * nseq * 2, BF16); t_cact = LS.trk()
        crep = LS.alloc(8 * nseq * 128 * 2, BF16); t_crep = LS.trk()
        bcada = LS.alloc(2048 * 4, F32); t_bcada = LS.trk()
        gtmp = LS.alloc(2048 * 4, F32); t_gtmp = LS.trk()
        stmp = LS.alloc(64 * 4, F32); t_stmp = LS.trk()

        psb = [es.enter_context(nc.psum_tensor("ps%d" % i, [128, 512], F32)) for i in range(4)]
        t_ps = [Trk() for _ in range(4)]
        psp = [es.enter_context(nc.psum_tensor("pp%d" % i, [128, 1024], F32)) for i in range(2)]
        t_pp = [[Trk(), Trk()] for _ in range(2)]
        rot = [0]

        def nxt():
            i = rot[0]
            rot[0] = (i + 1) % 4
            return psb[i], t_ps[i]

        def pvc(name, c, w=1, parts=128):
            o = pvo[name] + c
            return pvec[0:parts, o:o + w]

        def bcc(name, c=0, w=1):
            o = bco[name] + c
            return bcv[:, o:o + w]

        def v3(ap, a):
            return ap.rearrange("p (a b) -> p a b", a=a)

        def mm(out, lhsT, rhs, start, stop, reads, writes, chk=True, sig=False):
            if start and chk:
                for t in writes:
                    assert not (t.w and t.w[0][0] == "pe" and not t.r), "PSUM bank rewritten before it was read"
            R.op("pe", lambda e, o=out, l=lhsT, r=rhs, s=start, t=stop: e.matmul(o, l, r, start=s, stop=t),
                 reads=reads, writes=writes, signal=(stop or sig))

        def act(out, in_, func, reads, writes, bias=None, scale=None, accum=None):
            kw = {}
            if bias is not None:
                kw["bias"] = bias
            if scale is not None:
                kw["scale"] = scale
            if accum is not None:
                kw["accum_out"] = accum
            R.op("act", lambda e, o=out, i=in_, f=func, k=kw: e.activation(o, i, f, **k), reads=reads, writes=writes)

        def ts(eng, out, in0, s1, s2, op0, op1, reads, writes, accum=None):
            if op1 is None:
                R.op(eng, lambda e, o=out, i=in0, a=s1, p=op0: e.tensor_scalar(o, i, a, None, p),
                     reads=reads, writes=writes)
            elif accum is None:
                R.op(eng, lambda e, o=out, i=in0, a=s1, b=s2, p=op0, q=op1: e.tensor_scalar(o, i, a, b, p, q),
                     reads=reads, writes=writes)
            else:
                R.op(eng, lambda e, o=out, i=in0, a=s1, b=s2, p=op0, q=op1, c=accum:
                     e.tensor_scalar(o, i, a, b, p, q, accum_out=c), reads=reads, writes=writes)

        def tt(eng, out, in0, in1, op, reads, writes):
            R.op(eng, lambda e, o=out, a=in0, b=in1, p=op: e.tensor_tensor(o, a, b, p), reads=reads, writes=writes)

        def stt(out, in0, scalar, in1, op0, op1, reads, writes):
            R.op("dve", lambda e, o=out, a=in0, s=scalar, b=in1, p=op0, q=op1:
                 e.scalar_tensor_tensor(o, a, s, b, p, q), reads=reads, writes=writes)

        def cp(eng, out, in_, reads, writes):
            R.op(eng, lambda e, o=out, i=in_: e.tensor_copy(o, i), reads=reads, writes=writes)

        def mset(eng, ap, val, writes):
            R.op(eng, lambda e, a=ap, v=val: e.memset(a, v), writes=writes)

        def dma(q, out, in_, reads, writes, semtrk=None):
            R.dma(q, lambda e, o=out, i=in_: e.dma_start(out=o, in_=i), reads=reads, writes=writes, semtrk=semtrk)

        def cast_rows(dst, src, nrows, trk):
            r0 = 0
            while r0 < nrows:
                n = min(1024, nrows - r0)
                dma("pool", dst.ap()[r0:r0 + n, :], src.ap()[r0:r0 + n, :], [], [trk])
                r0 += n

        cast_rows(wb_in, win_d, NWG * 128, t_wb["in"])
        cast_rows(wb_oa, woa_d, 512, t_wb["oa"])
        cast_rows(wb_or, wor_d, 1024, t_wb["or"])
        cast_rows(wb_out, wout_d, 1024, t_wb["out"])
        cast_rows(wb_up, wup_d, NJ * 128, t_wb["up"])
        cast_rows(wb_down, wdown_d, DFF, t_wb["down"])
        dma("pool", wgate[:, :], wgate_d.ap(), [], [t_wgate])

        dma("sp", pvec[:, :], pvec_d.ap(), [], [t_pvec])
        dma("sp", bcv[:, :], bcv_d.ap(), [], [t_bcv])
        dma("sp", tbl[:, :], tbl_d.ap(), [], [t_tbl])
        dma("sp", cTs, cT_d.ap(), [], [t_cTs])
        dma("sp", bcada, bcada_d.ap(), [], [t_bcada])
        cp("dve", identb[:, :], tbl[:, 2048 + 128:2048 + 256], [t_tbl], [t_identb])
        mset("pool", ones64[:, :], 1.0, [t_ones64])
        mset("pool", onesf[:, :], 1.0, [t_onesf])
        mset("pool", small[:, :], 0.0, [])
        ts("dve", bq8[:, :], pvc("bq", 0, 8, 64), 0.125, None, ALU.mult, None, [t_pvec], [t_bq8])
        act(stmp[:, 0:8], pvc("lam", 0, 8), AF.Exp, [t_pvec], [t_stmp], scale=-1.0)
        act(stmp[:, 8:16], stmp[:, 0:8], AF.Ln, [t_stmp], [t_stmp], bias=1.0, scale=1.0)
        ts("dve", cneg[:, 0:8], stmp[:, 8:16], -8.0, None, ALU.mult, None, [t_stmp], [t_cneg])
        ts("dve", cneg[:, 8:16], stmp[:, 8:16], -16.0, None, ALU.mult, None, [t_stmp], [t_cneg])
        act(cact, cTs, AF.Silu, [t_cTs], [t_cact])
        cact3 = v3(cact, 8)
        crep4 = crep.rearrange("p (k b r) -> p k b r", k=8, b=nseq)
        for b in range(nseq):
            for kc in range(8):
                cp("pool", crep4[:, kc, b, :], cact3[:, kc, b:b + 1].to_broadcast([128, 128]), [t_cact], [t_crep])
        modT4 = modT[:, :].rearrange("p (k c b) -> p k c b", k=4, c=8)
        gs4 = gs[:, :].rearrange("p (k c b) -> p k c b", k=2, c=8)
        kinds = {0: ("sh1", 0), 1: ("sc1", 1), 3: ("sh2", 2), 4: ("sc2", 3)}
        wada_v = wada_d.ap().rearrange("(kc p) n -> p kc n", p=128)
        wst3 = v3(wst, 8)
        for cb in range(6):
            dma("pool", wst3, wada_v[:, :, cb * 1024:(cb + 1) * 1024], [], [t_wst])
            if cb in kinds:
                nm, ki = kinds[cb]
                ps, tp = nxt()
                for fc in range(8):
                    for kc in range(8):
                        mm(ps[:, fc * nseq:(fc + 1) * nseq], wst3[:, kc, fc * 128:(fc + 1) * 128], cact3[:, kc, :],
                           kc == 0, kc == 7, [t_wst, t_cact], [tp], chk=False)
                bap = pvc("ada_" + nm, 0, 8)
                for b in range(nseq):
                    tt("dve", modT4[:, ki, :, b], ps[:, 0:8 * nseq].rearrange("p (c b) -> p c b", c=8)[:, :, b], bap,
                       ALU.add, [tp, t_pvec], [t_modT])
            else:
                gi = 0 if cb == 2 else 1
                for b in range(nseq):
                    for half in range(2):
                        ps, tp = nxt()
                        for kc in range(8):
                            mm(ps[:, :], crep4[:, kc, b, :], wst3[:, kc, half * 512:(half + 1) * 512],
                               kc == 0, kc == 7, [t_wst, t_crep], [tp])
                        c0 = gi * 1024 + half * 512
                        tt("dve", gtmp[:, c0:c0 + 512], ps[:, :], bcada[:, c0:c0 + 512], ALU.add,
                           [tp, t_bcada], [t_gtmp])
                        dma("sp", gabc_d.ap()[b * 128:(b + 1) * 128, c0:c0 + 512], gtmp[:, c0:c0 + 512],
                            [t_gtmp], [t_gabc_d[b]], semtrk=t_gtmp)
        for wi_, (gname, ki) in enumerate((("gmix", 1), ("gffn", 3))):
            for b in range(nseq):
                ts("dve", gs4[:, wi_, :, b], modT4[:, ki, :, b], 1.0, None, ALU.add, None, [t_modT], [t_gs])
                tt("dve", gs4[:, wi_, :, b], gs4[:, wi_, :, b], pvc(gname, 0, 8), ALU.mult, [t_gs, t_pvec], [t_gs])
        R.fence(LS.trks, LM.trks)

        xnT3 = v3(xnT, 8)
        hnT3 = v3(hnT, 8)
        qT3 = v3(qT, 8)
        qiT3 = v3(qiT, 8)
        kT3 = v3(kT[:, :], 2)
        vaug4 = vaug[:, :].rearrange("p (j k c) -> p j k c", j=16, k=2)
        wis3 = v3(wis, 4)
        maskT3 = v3(maskT, 16)
        attnT3 = v3(attnT, 8)
        rnnT3 = v3(rnnT, 8)
        mergedT3 = v3(mergedT[:, :], 8)
        T01 = v3(tbl[:, 0:2048], 8)
        negmask = tbl[:, 2048:2048 + 128]
        hxr3 = v3(halo_xr[:, :], 8)
        hup3 = v3(halo_up[:, :], 44)
        wgate3 = v3(wgate[:, :], 16)
        wbin_v = wb_in.ap().rearrange("(g p) n -> g p n", p=128)
        wbup_v = wb_up.ap().rearrange("(g p) n -> g p n", p=128)
        wboa_v = wb_oa.ap().rearrange("(g p) n -> g p n", p=64)
        wbor_v = wb_or.ap().rearrange("(g p) n -> g p n", p=128)
        wbdown_v = wb_down.ap().rearrange("(j p) n -> p j n", p=128)
        wbout_v = wb_out.ap().rearrange("(kc p) n -> p kc n", p=128)
        WI_SCALE = float(64 ** -0.5 * 8 ** -0.5)

        def stage_in(name):
            i = stg_i[0]
            stg_i[0] = (i + 1) % NST
            dma("sp", stg[i], wbin_v[WIN_IDX[name]], [t_wb["in"]], [t_stg[i]])
            return v3(stg[i], 8), t_stg[i]

        def stage_raw(src_ap, parts=128):
            i = stg_i[0]
            stg_i[0] = (i + 1) % NST
            dma("sp", stg[i][0:parts, :], src_ap, [t_wb["oa"], t_wb["or"]], [t_stg[i]])
            return v3(stg[i][0:parts, :], 8), t_stg[i]

        def rms_scale(xt, t_x, junk, t_junk, xsb, t_xsb):
            ss, t_ss = smc("ss", 0)
            rs, t_rs = smc("rs", 1)
            act(junk, xt, AF.Square, [t_x], [t_junk, t_ss], accum=ss)
            ts("dve", rs, ss, 1.0 / D, EPS, ALU.mult, ALU.add, [t_ss], [t_rs])
            act(rs, rs, AF.Sqrt, [t_rs], [t_rs])
            R.op("dve", lambda e, o=rs: e.reciprocal(o, o), reads=[t_rs], writes=[t_rs])
            ts("dve", xsb, xt, rs, None, ALU.mult, None, [t_x, t_rs], [t_xsb])

        def norm_T(xt, t_x, junk, t_junk, xsl, t_xsl, which, b, dst3, t_dst, i):
            k = i % 2
            rms_scale(xt, t_x, junk, t_junk, xsl[k], t_xsl[k])
            ps, tp = nxt()
            psb16 = ps.bitcast(BF16)
            for c in range(8):
                R.op("pe", lambda e, o=psb16[:, c * 128:(c + 1) * 128], a=xsl[k][:, c * 128:(c + 1) * 128]:
                     e.transpose(o, a, identb[:, :]), reads=[t_xsl[k], t_identb], writes=[tp], signal=(c == 7))
            shk = 0 if which == 0 else 2
            for c in range(8):
                o = dst3[:, c, 128 * i:128 * i + 128]
                sc_ap = gs4[:, which, c, b:b + 1]
                sh_ap = modT4[:, shk, c, b:b + 1]
                if c % 2 == 0:
                    act(o, psb16[:, c * 128:(c + 1) * 128], AF.Identity, [tp, t_gs, t_modT], [t_dst], bias=sh_ap, scale=sc_ap)
                else:
                    ts("dve", o, psb16[:, c * 128:(c + 1) * 128], sc_ap, sh_ap, ALU.mult, ALU.add,
                       [tp, t_gs, t_modT], [t_dst])

        out_events = []

        for b in range(nseq):
            dma("sp", gabc[:, :], gabc_d.ap()[b * 128:(b + 1) * 128, :], t_gabc_d, [t_gabc])
            mset("pool", halo_xr[:, :], 0.0, t_hxr)
            mset("pool", halo_up[:, :], 0.0, t_hup)
            mset("pool", hl[:, :], 0.0, t_hl)
            mset("pool", kmaxsq[:, :], 0.0, [t_kmax])
            mset("pool", vaug[:, :], 1.0, [t_vaug])
            for g in range(nblk):
                t0 = TB * g
                for i in range(4):
                    dma("sp", xh[i][:, :], x_d.ap()[b, t0 + 128 * i:t0 + 128 * i + 128, :], [], [t_xh[i]])
                    norm_T(xh[i][:, :], t_xh[i], junkA, t_junkA, xs, t_xs, 0, b, xnT3, t_xnT[i], i)
                qmx, t_qmx = smc("qmx", 8, 8)
                kmb, t_kmb = smc("kmb", 16, 2)

                def proj_head(st3, tst, col0, dst, t_dst, bias_ap, scale, sq_dst=None):
                    ps, tp = nxt()
                    for kc in range(8):
                        mm(ps[0:64, :], st3[:, kc, col0:col0 + 64], xnT3[:, kc, :], kc == 0, kc == 7, [tst] + t_xnT, [tp])
                    act(dst, ps[0:64, :], AF.Identity, [tp, t_pvec, t_bq8], [t_dst], bias=bias_ap, scale=scale)
                    if sq_dst is not None:
                        k = sq_dst[2] % 2
                        act(sqt[k], ps[0:64, :], AF.Square, [tp, t_pvec, t_bq8], [t_sqt[k]], bias=bias_ap, scale=scale)
                        p2, tp2 = nxt()
                        mm(p2[:, :], ones64[:, :], sqt[k], True, True, [t_ones64, t_sqt[k]], [tp2])
                        R.op("dve", lambda e, o=sq_dst[0], a=p2[:, :]: e.reduce_max(o, a, AX.X),
                             reads=[tp2], writes=[sq_dst[1]])

                for cgi in range(4):
                    st3, tst = stage_in("q%d" % cgi)
                    for hh_ in range(2):
                        h = 2 * cgi + hh_
                        proj_head(st3, tst, 64 * hh_, qT3[:, h, :], t_q[h], bq8[:, h:h + 1], 0.125,
                                  (qmx[:, h:h + 1], t_qmx, h))
                st3, tst = stage_in("k")
                for kv in range(2):
                    proj_head(st3, tst, 64 * kv, kT3[:, kv, t0:t0 + TB], t_kT, pvc("bk", kv, 1, 64), 1.0,
                              (kmb[:, kv:kv + 1], t_kmb, kv))
                tt("dve", kmaxsq[:, :], kmaxsq[:, :], kmb, ALU.max, [t_kmb, t_kmax], [t_kmax])
                st3, tst = stage_in("v")
                for i in range(4):
                    ps, tp = nxt()
                    for kc in range(8):
                        mm(ps[:, 0:128], xnT3[:, kc, 128 * i:128 * i + 128], st3[:, kc, :], kc == 0, kc == 7,
                           [tst, t_xnT[i]], [tp])
                    tt("dve", vaug4[:, 4 * g + i, :, 0:64], v3(ps[:, 0:128], 2), v3(bcc("bv", 0, 128), 2), ALU.add,
                       [tp, t_bcv], [t_vaug])
                for cgi in range(4):
                    st3, tst = stage_in("qi%d" % cgi)
                    for hh_ in range(2):
                        h = 2 * cgi + hh_
                        proj_head(st3, tst, 64 * hh_, qiT3[:, h, :], t_qi[h], pvc("bqi", h, 1, 64), 1.0)
                st3, tst = stage_in("kiwi")
                proj_head(st3, tst, 0, kiT[:, t0:t0 + TB], t_kiT, pvc("bki", 0, 1, 64), 1.0)
                for i in range(4):
                    ps, tp = nxt()
                    for kc in range(8):
                        mm(ps[:, 0:8], xnT3[:, kc, 128 * i:128 * i + 128], st3[:, kc, 64:72], kc == 0, kc == 7,
                           [tst, t_xnT[i]], [tp])
                    tt("dve", wis3[:, i, :], ps[:, 0:8], bcc("bwi", 0, 8), ALU.add, [tp, t_bcv], [t_wis[i]])
                    ts("dve", wis3[:, i, :], wis3[:, i, :], WI_SCALE, None, ALU.mult, None, [t_wis[i]], [t_wis[i]])

                def rnn_front(n):
                    k = n % 2
                    st3, tst = stage_in("xr%d" % n)
                    ps, tp = nxt()
                    for kc in range(8):
                        mm(ps[:, :], st3[:, kc, :], xnT3[:, kc, :], kc == 0, kc == 7, [tst] + t_xnT, [tp])
                    cp("pool", xrb[k][:, 0:3], hxr3[:, n, :], [t_hxr[n]], [t_xrb[k]])
                    act(xrb[k][:, 3:515], ps[:, :], AF.Identity, [tp, t_pvec], [t_xrb[k]], bias=pvc("bxr", n), scale=1.0)
                    yield
                    ts("dve", xc[k], xrb[k][:, 0:512], pvc("crw", 4 * n), pvc("crb", n), ALU.mult, ALU.add,
                       [t_xrb[k], t_pvec], [t_xc[k]])
                    for kk in range(1, 4):
                        stt(xc[k], xrb[k][:, kk:kk + 512], pvc("crw", 4 * n + kk), xc[k], ALU.mult, ALU.add,
                            [t_xrb[k], t_pvec, t_xc[k]], [t_xc[k]])
                        yield
                    cp("pool", hxr3[:, n, :], xrb[k][:, 512:515], [t_xrb[k]], [t_hxr[n]])
                    cp("pool", xcb[k], xc[k], [t_xc[k]], [t_xcb[k]])
                    yield
                    st3, tst = stage_in("yr%d" % n)
                    yp, typ = nxt()
                    for kc in range(8):
                        mm(yp[:, :], st3[:, kc, :], xnT3[:, kc, :], kc == 0, kc == 7, [tst] + t_xnT, [typ])
                    act(yy[k], yp[:, :], AF.Identity, [typ, t_pvec], [t_yy[k]], bias=pvc("byr", n), scale=1.0)
                    yield

                    def back():
                        rp, trp = nxt()
                        mm(rp[:, :], wgate3[:, n, :], xcb[k], True, True, [t_wgate, t_xcb[k]], [trp])
                        ip, tip = nxt()
                        mm(ip[:, :], wgate3[:, 8 + n, :], xcb[k], True, True, [t_wgate, t_xcb[k]], [tip])
                        act(rr, rp[:, :], AF.Sigmoid, [trp, t_pvec], [t_rr], bias=pvc("brg", n), scale=1.0)
                        act(ii, ip[:, :], AF.Sigmoid, [tip, t_pvec], [t_ii], bias=pvc("big", n), scale=1.0)
                        yield
                        tt("pool", y2, yy[k], yy[k], ALU.mult, [t_yy[k]], [t_y2])
                        act(aa, rr, AF.Exp, [t_rr, t_cneg], [t_aa], scale=cneg[:, n:n + 1])
                        act(a2, rr, AF.Exp, [t_rr, t_cneg], [t_a2], scale=cneg[:, 8 + n:9 + n])
                        tt("dve", uu, xc[k], ii, ALU.mult, [t_xc[k], t_ii], [t_uu])
                        yield
                        act(a2, a2, AF.Sqrt, [t_a2], [t_a2], bias=1.0, scale=-1.0)
                        ts("pool", y2, y2, 0.044715, 1.0, ALU.mult, ALU.add, [t_y2], [t_y2])
                        tt("pool", y2, y2, yy[k], ALU.mult, [t_y2, t_yy[k]], [t_y2])
                        yield
                        tt("dve", uu, uu, a2, ALU.mult, [t_uu, t_a2], [t_uu])
                        act(y2, y2, AF.Sigmoid, [t_y2], [t_y2], scale=1.5957691216057308)
                        R.op("dve", lambda e, o=hh, a=aa, u=uu, i0=hl[:, n:n + 1]:
                             e.tensor_tensor_scan(o, a, u, i0, ALU.mult, ALU.add), reads=[t_aa, t_uu, t_hl[n]], writes=[t_hh])
                        yield
                        tt("pool", y2, y2, yy[k], ALU.mult, [t_y2, t_yy[k]], [t_y2])
                        cp("pool", hl[:, n:n + 1], hh[:, 511:512], [t_hh], [t_hl[n]])
                        tt("pool", rnnT3[:, n, :], hh, y2, ALU.mult, [t_hh, t_y2], [t_rnn[n]])
                        yield
                    return back

                def rnn_gen():
                    R.fence(merge_alias_trks, rnn_alias_trks)
                    prev = None
                    for n in range(8):
                        bk = yield from rnn_front(n)
                        if prev is not None:
                            yield from prev()
                        prev = bk
                    yield from prev()

                rgen = rnn_gen()
                rnn_done = [False]

                def pump(k=1):
                    for _ in range(k):
                        if rnn_done[0]:
                            return
                        try:
                            next(rgen)
                        except StopIteration:
                            rnn_done[0] = True

                n_points = sum(((128 * (4 * g + i + 1) + 511) // 512) * 8 for i in range(4)) + \
                    NIT * sum(1 for i in range(4) if 4 * g + i >= 2)
                RNN_YIELDS = 8 * 14
                ppp = -(-RNN_YIELDS // n_points)

                for i in range(4):
                    ti = 4 * g + i
                    nk = 128 * (ti + 1)
                    for kb in range(0, nk, 512):
                        w = min(512, nk - kb)
                        for h in range(8):
                            ps, tp = nxt()
                            mm(ps[:, 0:w], qiT3[:, h, 128 * i:128 * i + 128], kiT[:, kb:kb + w], True, True,
                               [t_qi[h], t_kiT], [tp])
                            k = h % 2
                            act(rl[k][:, 0:w], ps[:, 0:w], AF.Relu, [tp], [t_rl[k]])
                            if h == 0:
                                ts("dve", sc[:, kb:kb + w], rl[k][:, 0:w], wis3[:, i, 0:1], None, ALU.mult, None,
                                   [t_rl[k], t_wis[i]], [t_sc])
                            else:
                                stt(sc[:, kb:kb + w], rl[k][:, 0:w], wis3[:, i, h:h + 1], sc[:, kb:kb + w],
                                    ALU.mult, ALU.add, [t_rl[k], t_wis[i], t_sc], [t_sc])
                            pump(ppp)
                    if ti >= 2:
                        Rr, t_R = smc("R", 20)
                        lo, t_lo = smc("lo", 21)
                        mid, t_mid = smc("mid", 22)
                        cnt, t_cnt = smc("cnt", 23)
                        dd, t_dd = smc("dd", 24)
                        R.op("dve", lambda e, o=Rr, a=sc[:, 0:nk]: e.tensor_reduce(o, a, AX.X, ALU.max, apply_absolute_value=True),
                             reads=[t_sc], writes=[t_R])
                        tt("pool", sc[:, nk - 128:nk], sc[:, nk - 128:nk], negmask, ALU.add, [t_sc, t_tbl], [t_sc])
                        ts("dve", lo, Rr, -1.0, None, ALU.mult, None, [t_R], [t_lo])
                        for it in range(1, NIT + 1):
                            stp = float(2.0 ** (1 - it))
                            stt(mid, Rr, stp, lo, ALU.mult, ALU.add, [t_R, t_lo], [t_mid])
                            ts("dve", junkD[:, 0:nk], sc[:, 0:nk], mid, None, ALU.is_ge, ALU.add, [t_sc, t_mid],
                               [t_junkD, t_cnt], accum=cnt)
                            pump(ppp)
                            ts("dve", dd, cnt, NSEL - 0.5, stp, ALU.is_ge, ALU.mult, [t_cnt], [t_dd])
                            stt(lo, dd, Rr, lo, ALU.mult, ALU.add, [t_dd, t_R, t_lo], [t_lo])
                        ts("dve", mask[:, 0:nk], sc[:, 0:nk], lo, None, ALU.is_ge, None, [t_sc, t_lo], [t_mask])
                    else:
                        mset("pool", mask[:, 0:nk], 1.0, [t_mask])
                    j = 0
                    while j <= ti:
                        n = min(4, ti + 1 - j)
                        ps, tp = nxt()
                        psb16 = ps.bitcast(BF16)
                        for jj in range(n):
                            R.op("pe", lambda e, o=psb16[:, jj * 128:(jj + 1) * 128], a=mask[:, 128 * (j + jj):128 * (j + jj + 1)]:
                                 e.transpose(o, a, identb[:, :]), reads=[t_mask, t_identb], writes=[tp], signal=(jj == n - 1))
                        act(maskT3[:, j:j + n, 128 * i:128 * i + 128], v3(psb16[:, 0:n * 128], n), AF.Copy,
                            [tp], [t_maskT[i]])
                        j += n
                while not rnn_done[0]:
                    pump(1)

                nsb = 4 * g + 4
                prod, t_prod = smc("prod", 26, 8)
                negM, t_negM = smc("negM", 34, 8)
                negMf, t_negMf = smc("negMf", 42, 8)
                ts("dve", prod[:, 0:4], qmx[:, 0:4], kmaxsq[:, 0:1], None, ALU.mult, None, [t_qmx, t_kmax], [t_prod])
                ts("dve", prod[:, 4:8], qmx[:, 4:8], kmaxsq[:, 1:2], None, ALU.mult, None, [t_qmx, t_kmax], [t_prod])
                act(prod, prod, AF.Sqrt, [t_prod], [t_prod])
                ts("dve", negM, prod, -1.05, None, ALU.mult, None, [t_prod], [t_negM])
                tt("dve", negMf, negM, bcc("b31", 0, 8), ALU.add, [t_negM, t_bcv], [t_negMf])
                LA = 2
                steps = [(h, j) for h in range(8) for j in range(nsb)]

                def att_front(sidx):
                    h, j = steps[sidx]
                    kv = h // 4
                    d = j - 4 * g
                    c0 = 128 * max(d, 0)
                    lg, tlg = nxt()
                    mm(lg[:, c0:TB], kT3[:, kv, 128 * j:128 * j + 128], qT3[:, h, c0:TB], True, True,
                       [t_kT, t_q[h]], [tlg])
                    pt = PT[sidx % NPT]
                    tpt = t_PT[sidx % NPT]
                    n0 = max(d, 0)
                    n1 = min(d + 1, 3)
                    if d >= -1 and n0 <= n1:
                        ca, cb_ = 128 * n0, 128 * (n1 + 1)
                        ta = 128 * (n0 - d)
                        k = sidx % 3
                        tt("dve", nr[k][:, 0:cb_ - ca], lg[:, ca:cb_], T01[:, h, ta:ta + (cb_ - ca)], ALU.add,
                           [tlg, t_tbl], [t_nr[k]])
                        act(pt[:, ca:cb_], nr[k][:, 0:cb_ - ca], AF.Exp, [t_nr[k], t_negM], [tpt],
                            bias=negM[:, h:h + 1], scale=1.0)
                        fc0 = cb_
                    else:
                        fc0 = 0
                    if fc0 < TB:
                        act(pt[:, fc0:TB], lg[:, fc0:TB], AF.Exp, [tlg, t_negMf], [tpt],
                            bias=negMf[:, h:h + 1], scale=1.0)
                    tt("pool", pt[:, c0:TB], pt[:, c0:TB], maskT3[:, j, c0:TB], ALU.mult, [tpt] + t_maskT, [tpt])

                def att_back(sidx):
                    h, j = steps[sidx]
                    kv = h // 4
                    d = j - 4 * g
                    c0 = 128 * max(d, 0)
                    acc = psp[h % 2]
                    tacc = t_pp[h % 2][0]
                    pt = PT[sidx % NPT]
                    tpt = t_PT[sidx % NPT]
                    mm(acc[0:65, c0:TB], vaug4[:, j, kv, :], pt[:, c0:TB], j == 0, j == nsb - 1,
                       [t_vaug, tpt], [tacc], sig=True)
                    if j == nsb - 1:
                        k = h % 2
                        act(osb[k], acc[0:65, 0:TB], AF.Copy, [tacc], [t_osb[k]])
                        R.op("dve", lambda e, o=osb[k][64:65, :]: e.reciprocal(o, o), reads=[t_osb[k]], writes=[t_osb[k]])
                        bc_, tbc = nxt()
                        mm(bc_[0:64, :], onesf[64:65, 0:64], osb[k][64:65, :], True, True, [t_onesf, t_osb[k]], [tbc])
                        tt("dve", attnT3[:, h, :], osb[k][0:64, :], bc_[0:64, :], ALU.mult, [t_osb[k], tbc], [t_attn[h]])

                for sidx in range(len(steps) + LA):
                    if sidx < len(steps):
                        att_front(sidx)
                    if sidx >= LA:
                        att_back(sidx - LA)

                R.fence(rnn_alias_trks, merge_alias_trks)
                for m in range(8):
                    A, tA = psp[m % 2][:, 0:512], t_pp[m % 2][0]
                    Rp, tRp = psp[m % 2][:, 512:1024], t_pp[m % 2][1]
                    st3, tst = stage_raw(wboa_v[m], 64)
                    for h in range(8):
                        mm(A, st3[:, h, :], attnT3[:, h, :], h == 0, h == 7, [tst, t_attn[h]], [tA])
                    st3, tst = stage_raw(wbor_v[m])
                    for kc in range(8):
                        mm(Rp, st3[:, kc, :], rnnT3[:, kc, :], kc == 0, kc == 7, [tst, t_rnn[kc]], [tRp])
                    st3, tst = stage_in("ga%d" % m)
                    gp, tgp = nxt()
                    for kc in range(8):
                        mm(gp[:, :], st3[:, kc, :], xnT3[:, kc, :], kc == 0, kc == 7, [tst] + t_xnT, [tgp])
                    act(sa, gp[:, :], AF.Sigmoid, [tgp, t_pvec], [t_sa], bias=pvc("bga", m), scale=1.0)
                    st3, tst = stage_in("gb%d" % m)
                    gp, tgp = nxt()
                    for kc in range(8):
                        mm(gp[:, :], st3[:, kc, :], xnT3[:, kc, :], kc == 0, kc == 7, [tst] + t_xnT, [tgp])
                    act(sbg, gp[:, :], AF.Sigmoid, [tgp, t_pvec], [t_sbg], bias=pvc("bgb", m), scale=1.0)
                    tt("dve", t1, A, sa, ALU.mult, [tA, t_sa], [t_t1])
                    tt("dve", t2, Rp, sbg, ALU.mult, [tRp, t_sbg], [t_t2])
                    tt("pool", mergedT3[:, m, :], t1, t2, ALU.add, [t_t1, t_t2], [t_merged[m]])

                R.fence(LM.trks, LF.trks)
                dma("sp", v3(wout_s, 8), wbout_v, [t_wb["out"]], [t_wout])
                wout3 = v3(wout_s, 8)
                for i in range(4):
                    pp, tpp = psp[i % 2], t_pp[i % 2]
                    for half in range(2):
                        for m in range(8):
                            mm(pp[:, half * 512:(half + 1) * 512], mergedT3[:, m, 128 * i:128 * i + 128],
                               wout3[:, m, half * 512:(half + 1) * 512], m == 0, m == 7, [t_merged[m], t_wout], [tpp[half]])
                    tt("dve", dtmp, pp[:, :], gabc[:, 0:1024], ALU.mult, tpp + [t_gabc], [t_dtmp])
                    tt("pool", xh[i][:, :], dtmp, xh[i][:, :], ALU.add, [t_dtmp, t_xh[i]], [t_xh[i]])
                    norm_T(xh[i][:, :], t_xh[i], junkF, t_junkF, xs2, t_xs2, 1, b, hnT3, t_hnT[i], i)
                ub_i = 0
                for gi, js in enumerate(FF_GROUPS):
                    k2 = gi % 2
                    wd3 = v3(wd_s[k2], 6)
                    act3 = v3(actb[k2], 6)
                    dma("sp", wd3[:, 0:len(js), :], wbdown_v[:, js[0]:js[0] + len(js), :], [t_wb["down"]], [t_wd[k2]])
                    for jj, j in enumerate(js):
                        si = stu_i[0]
                        stu_i[0] = (si + 1) % NSU
                        dma("sp", stu[si], wbup_v[j], [t_wb["up"]], [t_stu[si]])
                        su3 = v3(stu[si], 8)
                        u = ub_i % 2
                        ub_i += 1
                        for (half, ub, tub, cc, tcc, jx) in ((0, ubv[u], t_ubv[u], cv[u], t_cv[u], j),
                                                           (1, ubg[u], t_ubg[u], cg[u], t_cg[u], NJ + j)):
                            ps, tp = nxt()
                            for kc in range(8):
                                mm(ps[:, :], su3[:, kc, half * 128:(half + 1) * 128], hnT3[:, kc, :], kc == 0, kc == 7,
                                   [t_stu[si]] + t_hnT, [tp])
                            cp("pool", ub[:, 0:2], hup3[:, jx, :], [t_hup[jx]], [tub])
                            act(ub[:, 2:514], ps[:, :], AF.Copy, [tp], [tub])
                            act(cc, ps[:, :], AF.Identity, [tp, t_pvec], [tcc], bias=pvc("cfb", jx), scale=pvc("cfw", 3 * jx + 2))
                            for k in range(0, 2):
                                stt(cc, ub[:, k:k + 512], pvc("cfw", 3 * jx + k), cc, ALU.mult, ALU.add,
                                    [tub, t_pvec, tcc], [tcc])
                            cp("pool", hup3[:, jx, :], ub[:, 512:514], [tub], [t_hup[jx]])
                        act(sgb[u], cg[u], AF.Silu, [t_cg[u]], [t_sg[u]])
                        tt("pool", act3[:, jj, :], sgb[u], cv[u], ALU.mult, [t_sg[u], t_cv[u]], [t_act[k2]])
                    for i in range(4):
                        pp, tpp = psp[i % 2], t_pp[i % 2]
                        for half in range(2):
                            for jj in range(len(js)):
                                mm(pp[:, half * 512:(half + 1) * 512], act3[:, jj, 128 * i:128 * i + 128],
                                   wd3[:, jj, half * 512:(half + 1) * 512], jj == 0, jj == len(js) - 1,
                                   [t_act[k2], t_wd[k2]], [tpp[half]])
                        tt("dve", dtmp, pp[:, :], gabc[:, 1024:2048], ALU.mult, tpp + [t_gabc], [t_dtmp])
                        tt("pool", xh[i][:, :], dtmp, xh[i][:, :], ALU.add, [t_dtmp, t_xh[i]], [t_xh[i]])
                for i in range(4):
                    ss, t_ss = smc("ss", 0)
                    rs, t_rs = smc("rs", 1)
                    act(junkF, xh[i][:, :], AF.Square, [t_xh[i]], [t_junkF, t_ss], accum=ss)
                    ts("dve", rs, ss, 1.0 / D, EPS, ALU.mult, ALU.add, [t_ss], [t_rs])
                    act(rs, rs, AF.Sqrt, [t_rs], [t_rs])
                    R.op("dve", lambda e, o=rs: e.reciprocal(o, o), reads=[t_rs], writes=[t_rs])
                    k = i % 2
                    stt(otile[k], xh[i][:, :], rs, bcc("gfin", 0, 1024), ALU.mult, ALU.mult, [t_xh[i], t_rs, t_bcv], [t_ot[k]])
                    dma("sp", out_d.ap()[b, t0 + 128 * i:t0 + 128 * i + 128, :], otile[k], [t_ot[k]], [])
                R.fence(LF.trks, LM.trks)

        R.wait_all("sp", t_ot)
        R.wait_all("sp", t_xh)

        assert not any(R.pending.values()), R.pending
        def replay(name, e):
            for it in R.streams[name]:
                if it[0] == "w":
                    e.wait_ge(sems[it[1]], it[2])
                elif it[0] == "i":
                    it[1](e).then_inc(sems[name], 1)
                elif it[0] == "n":
                    it[1](e)
                else:
                    it[1](e).then_inc(sems[it[2]], 16)

        with nc.Block() as block:
            @block.tensor
            def _(e):
                replay("pe", e)

            @block.scalar
            def _(e):
                replay("act", e)

            @block.vector
            def _(e):
                replay("dve", e)

            @block.gpsimd
            def _(e):
                replay("pool", e)

            @block.sync
            def _(e):
                replay("sp", e)
    return nc


def _PVN():
    pvo, _ = _offsets()
    return max(pvo.values()) + 32


def make_in_maps(inputs, nseq=4, ncores=NCORES):
    inp = {k: np.asarray(v, np.float32) for k, v in inputs.items()}
    sh, _, _ = _layout_shared(inp)
    assert sh["pvec"].shape[1] <= _PVN()
    pv = np.zeros((128, _PVN()), np.float32)
    pv[:, :sh["pvec"].shape[1]] = sh["pvec"]
    sh["pvec"] = pv
    maps = []
    for c in range(ncores):
        xs_ = np.ascontiguousarray(inp["x"][c * nseq:(c + 1) * nseq])
        cc = inp["c"][c * nseq:(c + 1) * nseq]
        cT = np.ascontiguousarray(cc.reshape(nseq, 8, 128).transpose(2, 1, 0)).reshape(128, 8 * nseq)
        m = dict(sh)
        m["x"] = xs_
        m["cT"] = np.ascontiguousarray(cT)
        maps.append(m)
    return maps


_NC_CACHE = {}


def kernel(**inputs):
    nseq = 4
    if nseq not in _NC_CACHE:
        _NC_CACHE[nseq] = build_nc(nseq)
    nc = _NC_CACHE[nseq]
    maps = make_in_maps(inputs, nseq, NCORES)
    res = run_bass_kernel_spmd(nc, maps, core_ids=list(range(NCORES)))
    out = np.concatenate([np.asarray(r["out"], np.float32).reshape(nseq, S, D) for r in res.results], axis=0)
    return out
```

```python
import math
from contextlib import ExitStack
import numpy as np
import concourse.bass as bass
import concourse.mybir as mybir
from concourse.bass_utils import run_bass_kernel_spmd

F32 = mybir.dt.float32
BF16 = mybir.dt.bfloat16
U8 = mybir.dt.uint8
I8 = mybir.dt.int8
AF = mybir.ActivationFunctionType
ALU = mybir.AluOpType
AX = mybir.AxisListType

NCORES = 8
S = 2048
D = 1024
NH = 8
DFF = 2816
NJ = 22
TB = 512
NBLK = S // TB
N_IN = 5448
EPS = 1e-6
NIT = 16
NSEL = 256
FF_GROUPS = [list(range(0, 6)), list(range(6, 12)), list(range(12, 17)), list(range(17, 22))]
WIN_GROUPS = ([("q%d" % i, 128 * i) for i in range(4)] + [("k", 512), ("v", 640)]
              + [("qi%d" % i, 768 + 128 * i) for i in range(4)] + [("kiwi", 1280)]
              + [("xr%d" % i, 1352 + 128 * i) for i in range(8)]
              + [("yr%d" % i, 2376 + 128 * i) for i in range(8)]
              + [("ga%d" % i, 3400 + 128 * i) for i in range(8)]
              + [("gb%d" % i, 4424 + 128 * i) for i in range(8)])
WIN_IDX = {n: i for i, (n, _) in enumerate(WIN_GROUPS)}
NWG = len(WIN_GROUPS)


class Trk:
    __slots__ = ("w", "r", "dsem", "dcnt", "name")

    def __init__(self, name=""):
        self.w = []
        self.r = []
        self.dsem = None
        self.dcnt = 0
        self.name = name

    def events(self):
        return list(self.w) + list(self.r)


ENGS = ("pe", "act", "dve", "pool", "sp")


class Rec:
    def __init__(self, n_dma_sems):
        self.streams = {e: [] for e in ENGS}
        self.count = {e: 0 for e in ENGS}
        self.waited = {e: {} for e in ENGS}
        self.n_dma_sems = n_dma_sems
        self.next_dsem = 0
        self.pending = {e: False for e in ENGS}

    def _waits(self, eng, deps):
        st = self.streams[eng]
        wd = self.waited[eng]
        for (sk, v) in deps:
            if eng == "pe" and sk == "pe":
                continue
            if wd.get(sk, 0) < v:
                st.append(("w", sk, v))
                wd[sk] = v

    @staticmethod
    def _deps(reads, writes):
        deps = []
        for t in reads:
            deps += t.w
        for t in writes:
            deps += t.w
            deps += t.r
        return deps

    @staticmethod
    def _commit(ev, reads, writes):
        for t in writes:
            t.w = [ev]
            t.r = []
        for t in reads:
            if t in writes:
                continue
            t.r = [e for e in t.r if e[0] != ev[0]] + [ev]

    def op(self, eng, fn, reads=(), writes=(), signal=True):
        self._waits(eng, self._deps(reads, writes))
        if signal:
            self.count[eng] += 1
            ev = (eng, self.count[eng])
            self.streams[eng].append(("i", fn))
            self.pending[eng] = False
        else:
            ev = (eng, self.count[eng] + 1)
            self.streams[eng].append(("n", fn))
            self.pending[eng] = True
        self._commit(ev, reads, writes)

    def dma(self, q, fn, reads=(), writes=(), semtrk=None):
        self._waits(q, self._deps(reads, writes))
        t = semtrk if semtrk is not None else (writes[0] if writes else reads[0])
        if t.dsem is None:
            assert self.next_dsem < self.n_dma_sems, "out of dma sems"
            t.dsem = "d%d" % self.next_dsem
            self.next_dsem += 1
        t.dcnt += 16
        ev = (t.dsem, t.dcnt)
        self.streams[q].append(("d", fn, t.dsem))
        self._commit(ev, reads, writes)

    def wait_all(self, eng, trks):
        deps = []
        for t in trks:
            deps += t.events()
        self._waits(eng, deps)

    @staticmethod
    def fence(old, new):
        evs = {}
        for t in old:
            for (sk, v) in t.events():
                if evs.get(sk, 0) < v:
                    evs[sk] = v
        for t in new:
            cur = {}
            for (sk, v) in t.events() + list(evs.items()):
                if cur.get(sk, 0) < v:
                    cur[sk] = v
            t.r = list(cur.items())


def _t5_bucket_np(n):
    n = np.maximum(n, 0)
    nf = np.maximum(n, 1).astype(np.float32)
    large = 16 + (np.log(nf / np.float32(16)) / np.float32(math.log(128 / 16)) * np.float32(16)).astype(np.int32)
    large = np.minimum(large, 31)
    return np.where(n < 16, n, large)


class VecPack:
    def __init__(self):
        self.cols = []
        self.off = {}
        self.n = 0

    def add(self, name, arr):
        arr = np.asarray(arr, np.float32)
        assert arr.shape[0] == 128
        self.off[name] = self.n
        self.cols.append(arr)
        self.n += arr.shape[1]

    def pack(self):
        return np.ascontiguousarray(np.concatenate(self.cols, axis=1))


def _fm(v, nch):
    v = np.asarray(v, np.float32).reshape(nch, 128)
    return v.T


def _pad_rows64(a):
    out = np.zeros((128, a.shape[1]), np.float32)
    out[:64] = a
    return out


def _layout_shared(inp):
    w_in = inp["w_in"][0]
    b_in = inp["b_in"][0]
    sh = {}
    wt = np.zeros((NWG, 128, 8, 128), np.float32)
    for gi, (name, c0) in enumerate(WIN_GROUPS):
        wdt = 72 if name == "kiwi" else 128
        blk = w_in[:, c0:c0 + wdt].reshape(8, 128, wdt)
        wt[gi, :, :, :wdt] = blk.transpose(1, 0, 2)
    sh["win_t"] = wt.reshape(NWG * 128, 1024)
    w_up = inp["w_up"][0]
    wu = np.zeros((NJ, 128, 8, 256), np.float32)
    for j in range(NJ):
        wu[j, :, :, 0:128] = w_up[:, 128 * j:128 * j + 128].reshape(8, 128, 128).transpose(1, 0, 2)
        wu[j, :, :, 128:256] = w_up[:, DFF + 128 * j:DFF + 128 * j + 128].reshape(8, 128, 128).transpose(1, 0, 2)
    sh["wup_t"] = wu.reshape(NJ * 128, 2048)
    sh["w_down"] = np.ascontiguousarray(inp["w_down"][0])
    woa = inp["w_o_attn"][0].reshape(8, 64, 8, 128)
    sh["woattn_t"] = np.ascontiguousarray(woa.transpose(2, 1, 0, 3)).reshape(8 * 64, 1024)
    wor = inp["w_o_rnn"][0].reshape(8, 128, 8, 128)
    sh["wornn_t"] = np.ascontiguousarray(wor.transpose(2, 1, 0, 3)).reshape(8 * 128, 1024)
    sh["w_out"] = np.ascontiguousarray(inp["w_out"][0])
    wg = np.concatenate([inp["w_rg"][0].transpose(1, 0, 2).reshape(128, 1024),
                         inp["w_ig"][0].transpose(1, 0, 2).reshape(128, 1024)], axis=1)
    sh["wgate_t"] = np.ascontiguousarray(wg)
    sh["w_ada"] = np.ascontiguousarray(inp["w_ada"][0])

    pv = VecPack()
    pv.add("gmix", _fm(inp["g_mix"][0], 8))
    pv.add("gffn", _fm(inp["g_ffn"][0], 8))
    pv.add("bq", _pad_rows64(b_in[0:512].reshape(8, 64).T))
    pv.add("bk", _pad_rows64(b_in[512:640].reshape(2, 64).T))
    pv.add("bqi", _pad_rows64(b_in[768:1280].reshape(8, 64).T))
    pv.add("bki", _pad_rows64(b_in[1280:1344].reshape(1, 64).T))
    pv.add("bxr", _fm(b_in[1352:2376], 8))
    pv.add("byr", _fm(b_in[2376:3400], 8))
    pv.add("bga", _fm(b_in[3400:4424], 8))
    pv.add("bgb", _fm(b_in[4424:5448], 8))
    crw = inp["conv_rnn_w"][0]
    pv.add("crw", crw.reshape(4, 8, 128).transpose(2, 1, 0).reshape(128, 32))
    pv.add("crb", _fm(inp["conv_rnn_b"][0], 8))
    pv.add("brg", _fm(inp["b_rg"][0], 8))
    pv.add("big", _fm(inp["b_ig"][0], 8))
    pv.add("lam", _fm(inp["lru_lambda"][0], 8))
    cfw = inp["conv_ffn_w"][0]
    pv.add("cfw", cfw.reshape(3, 44, 128).transpose(2, 1, 0).reshape(128, 132))
    pv.add("cfb", _fm(inp["conv_ffn_b"][0], 44))
    b_ada = inp["b_ada"][0]
    for nm, blk in (("sh1", 0), ("sc1", 1), ("sh2", 3), ("sc2", 4)):
        pv.add("ada_" + nm, _fm(b_ada[blk * 1024:(blk + 1) * 1024], 8))
    sh["pvec"] = pv.pack()

    bc = VecPack()
    rep = lambda v: np.broadcast_to(np.asarray(v, np.float32)[None, :], (128, len(v)))
    bc.add("bv", rep(b_in[640:768]))
    bc.add("bwi", rep(b_in[1344:1352]))
    bc.add("b31", rep(inp["rel_bias"][31, :]))
    bc.add("gfin", rep(inp["g_final"]))
    sh["bcv"] = bc.pack()
    sh["bcada"] = np.ascontiguousarray(np.concatenate([rep(b_ada[2048:3072]), rep(b_ada[5120:6144])], axis=1))

    rb = inp["rel_bias"]
    sl = np.arange(128)[:, None]
    tl = np.arange(128)[None, :]
    d0 = tl - sl
    d1 = 128 + tl - sl
    T = np.zeros((128, 8, 256), np.float32)
    b0 = rb[_t5_bucket_np(d0)]
    b1 = rb[_t5_bucket_np(d1)]
    T[:, :, 0:128] = np.where((d0 >= 0)[:, :, None], b0, np.float32(-30000.0)).transpose(0, 2, 1)
    T[:, :, 128:256] = b1.transpose(0, 2, 1)
    negm = np.where(np.arange(128)[None, :] <= np.arange(128)[:, None], 0.0, -1e30).astype(np.float32)
    ident = np.eye(128, dtype=np.float32)
    sh["tbl"] = np.ascontiguousarray(np.concatenate([T.reshape(128, 2048), negm, ident], axis=1))
    return sh, pv.off, bc.off


PV_OFF = None
BC_OFF = None


def _offsets():
    global PV_OFF, BC_OFF
    if PV_OFF is None:
        z = lambda *s: np.zeros(s, np.float32)
        fake = {"w_in": z(1, 1024, N_IN), "b_in": z(1, N_IN), "w_up": z(1, 1024, 2 * DFF), "w_down": z(1, DFF, 1024),
                "w_o_attn": z(1, 512, 1024), "w_o_rnn": z(1, 1024, 1024), "w_out": z(1, 1024, 1024),
                "w_rg": z(1, 8, 128, 128), "w_ig": z(1, 8, 128, 128), "w_ada": z(1, 1024, 6144),
                "g_mix": z(1, 1024), "g_ffn": z(1, 1024), "conv_rnn_w": z(1, 4, 1024), "conv_rnn_b": z(1, 1024),
                "b_rg": z(1, 1024), "b_ig": z(1, 1024), "lru_lambda": z(1, 1024), "conv_ffn_w": z(1, 3, 2 * DFF),
                "conv_ffn_b": z(1, 2 * DFF), "b_ada": z(1, 6144), "rel_bias": z(32, 8), "g_final": z(1024)}
        _, PV_OFF, BC_OFF = _layout_shared(fake)
    return PV_OFF, BC_OFF


def build_nc(nseq=4, nblk=NBLK, dbg=False):
    pvo, bco = _offsets()
    NPV = max(pvo.values()) + 64
    nc = bass.Bass("TRN2", target_bir_lowering=False)
    R = Rec(n_dma_sems=56)

    def din(name, shape, dt=F32):
        return nc.dram_tensor(name, list(shape), dt, kind="ExternalInput")

    x_d = din("x", [nseq, S, D])
    cT_d = din("cT", [128, 8 * nseq])
    wada_d = din("w_ada", [1024, 6144])
    pvec_d = din("pvec", [128, _PVN()])
    bcv_d = din("bcv", [128, 128 + 8 + 8 + 1024])
    bcada_d = din("bcada", [128, 2048])
    tbl_d = din("tbl", [128, 2048 + 256])
    win_d = din("win_t", [NWG * 128, 1024])
    wup_d = din("wup_t", [NJ * 128, 2048])
    wdown_d = din("w_down", [DFF, 1024])
    woa_d = din("woattn_t", [512, 1024])
    wor_d = din("wornn_t", [1024, 1024])
    wout_d = din("w_out", [1024, 1024])
    wgate_d = din("wgate_t", [128, 2048])
    out_d = nc.dram_tensor("out", [nseq, S, D], F32, kind="ExternalOutput")
    wb_in = nc.dram_tensor("wb_in", [NWG * 128, 1024], BF16)
    wb_up = nc.dram_tensor("wb_up", [NJ * 128, 2048], BF16)
    wb_down = nc.dram_tensor("wb_down", [DFF, 1024], BF16)
    wb_oa = nc.dram_tensor("wb_oa", [512, 1024], BF16)
    wb_or = nc.dram_tensor("wb_or", [1024, 1024], BF16)
    wb_out = nc.dram_tensor("wb_out", [1024, 1024], BF16)
    gabc_d = nc.dram_tensor("gabc_d", [nseq * 128, 2048], F32)
    t_wb = {k: Trk("wb_" + k) for k in ("in", "up", "down", "oa", "or", "out")}
    t_gabc_d = [Trk("gabc_d%d" % b) for b in range(nseq)]

    es = ExitStack()
    with es:
        es.enter_context(nc.allow_low_precision("bf16 matmul operands, fp32 accumulation (problem tolerance)"))

        def sb(name, shape, dt):
            return es.enter_context(nc.sbuf_tensor("sb_" + name, list(shape), dt))

        sems = {}
        for e in ENGS:
            sems[e] = es.enter_context(nc.semaphore("s_" + e))
        for i in range(R.n_dma_sems):
            sems["d%d" % i] = es.enter_context(nc.semaphore("s_d%d" % i))

        pvec = sb("pvec", [128, _PVN()], F32); t_pvec = Trk()
        bcv = sb("bcv", [128, 128 + 8 + 8 + 1024], F32); t_bcv = Trk()
        tbl = sb("tbl", [128, 2048 + 256], F32); t_tbl = Trk()
        identb = sb("identb", [128, 128], BF16); t_identb = Trk()
        ones64 = sb("ones64", [64, 128], BF16); t_ones64 = Trk()
        onesf = sb("onesf", [128, 64], F32); t_onesf = Trk()
        wgate = sb("wgate", [128, 2048], BF16); t_wgate = Trk()
        modT = sb("modT", [128, 4 * 8 * nseq], F32); t_modT = Trk()
        gs = sb("gs", [128, 2 * 8 * nseq], F32); t_gs = Trk()
        cneg = sb("cneg", [128, 16], F32); t_cneg = Trk()
        bq8 = sb("bq8", [64, 8], F32); t_bq8 = Trk()
        gabc = sb("gabc", [128, 2048], F32); t_gabc = Trk()
        kT = sb("kT", [64, 2 * S], BF16); t_kT = Trk()
        vaug = sb("vaug", [128, 16 * 2 * 65], BF16); t_vaug = Trk()
        kiT = sb("kiT", [64, S], BF16); t_kiT = Trk()
        halo_xr = sb("halo_xr", [128, 8 * 3], F32); t_hxr = [Trk() for _ in range(8)]
        halo_up = sb("halo_up", [128, 44 * 2], F32); t_hup = [Trk() for _ in range(44)]
        hl = sb("hl", [128, 8], F32); t_hl = [Trk() for _ in range(8)]
        kmaxsq = sb("kmaxsq", [128, 2], F32); t_kmax = Trk()
        xh = [sb("xh%d" % i, [128, D], F32) for i in range(4)]; t_xh = [Trk() for _ in range(4)]
        mergedT = sb("mergedT", [128, 8 * TB], BF16); t_merged = [Trk() for _ in range(8)]
        small = sb("small", [128, 64], F32)
        t_small = {}

        def smc(name, c0, w=1):
            if name not in t_small:
                t_small[name] = (Trk(name), c0, w)
            t, c, ww = t_small[name]
            return small[:, c:c + ww], t

        ARENA_BYTES = 130 * 1024
        arena = sb("arena", [128, ARENA_BYTES // 2], BF16)
        ar_f32 = arena.bitcast(F32)
        ar_u8 = arena.bitcast(U8)
        ar_i8 = arena.bitcast(I8)

        class Lay:
            def __init__(self):
                self.off = 0
                self.trks = []

            def alloc(self, nbytes, dt, parts=128):
                o = self.off
                self.off += (nbytes + 63) // 64 * 64
                assert self.off <= ARENA_BYTES, ("arena overflow", self.off)
                h = {BF16: arena, F32: ar_f32, U8: ar_u8, I8: ar_i8}[dt]
                esz = {BF16: 2, F32: 4, U8: 1, I8: 1}[dt]
                return h[0:parts, o // esz:(o + nbytes) // esz]

            def trk(self, name=""):
                t = Trk(name)
                self.trks.append(t)
                return t

        LM = Lay()
        xnT = LM.alloc(8 * TB * 2, BF16); t_xnT = [LM.trk() for _ in range(4)]
        xs = [LM.alloc(D * 2, BF16) for _ in range(2)]; t_xs = [LM.trk() for _ in range(2)]
        junkA = LM.alloc(D * 2, BF16); t_junkA = LM.trk()
        qT = LM.alloc(8 * TB * 2, BF16, 64); t_q = [LM.trk() for _ in range(8)]
        qiT = LM.alloc(8 * TB * 2, BF16, 64); t_qi = [LM.trk() for _ in range(8)]
        sqt = [LM.alloc(TB * 2, BF16, 64) for _ in range(2)]; t_sqt = [LM.trk() for _ in range(2)]
        wis = LM.alloc(4 * 8 * 4, F32); t_wis = [LM.trk() for _ in range(4)]
        sc = LM.alloc(S * 4, F32); t_sc = LM.trk()
        rl = [LM.alloc(TB * 4, F32) for _ in range(2)]; t_rl = [LM.trk() for _ in range(2)]
        _mo = LM.off
        mask = LM.alloc(S * 2, BF16); t_mask = LM.trk()
        junkD = ar_u8[:, _mo:_mo + S]; t_junkD = t_mask
        junkS = ar_i8[:, _mo + S:_mo + 2 * S]; t_junkS = t_mask
        maskT = LM.alloc(16 * TB * 2, BF16); t_maskT = [LM.trk() for _ in range(4)]
        NPT = 4
        PT = [LM.alloc(TB * 2, BF16) for _ in range(NPT)]; t_PT = [LM.trk() for _ in range(NPT)]
        nr = [LM.alloc(256 * 4, F32) for _ in range(3)]; t_nr = [LM.trk() for _ in range(3)]
        osb = [LM.alloc(TB * 4, F32, 65) for _ in range(2)]; t_osb = [LM.trk() for _ in range(2)]
        attnT = LM.alloc(8 * TB * 2, BF16, 64); t_attn = [LM.trk() for _ in range(8)]
        xrb = [LM.alloc(516 * 4, F32) for _ in range(2)]; t_xrb = [LM.trk() for _ in range(2)]
        xc = [LM.alloc(TB * 4, F32) for _ in range(2)]; t_xc = [LM.trk() for _ in range(2)]
        xcb = [LM.alloc(TB * 2, BF16) for _ in range(2)]; t_xcb = [LM.trk() for _ in range(2)]
        rnn_tmp_off = LM.off
        rr = LM.alloc(TB * 4, F32); t_rr = LM.trk()
        ii = LM.alloc(TB * 4, F32); t_ii = LM.trk()
        aa = LM.alloc(TB * 4, F32); t_aa = LM.trk()
        a2 = LM.alloc(TB * 4, F32); t_a2 = LM.trk()
        rnn_alias_trks = [t_rr, t_ii, t_aa, t_a2]
        uu = LM.alloc(TB * 4, F32); t_uu = LM.trk()
        hh = LM.alloc(TB * 4, F32); t_hh = LM.trk()
        yy = [LM.alloc(TB * 4, F32) for _ in range(2)]; t_yy = [LM.trk() for _ in range(2)]
        y2 = LM.alloc(TB * 4, F32); t_y2 = LM.trk()
        rnnT = LM.alloc(8 * TB * 2, BF16); t_rnn = [LM.trk() for _ in range(8)]
        _save = LM.off
        LM.off = rnn_tmp_off
        sa = LM.alloc(TB * 4, F32); t_sa = LM.trk()
        sbg = LM.alloc(TB * 4, F32); t_sbg = LM.trk()
        t1 = LM.alloc(TB * 4, F32); t_t1 = LM.trk()
        t2 = LM.alloc(TB * 4, F32); t_t2 = LM.trk()
        merge_alias_trks = [t_sa, t_sbg, t_t1, t_t2]
        LM.off = _save
        NST = 4
        stg = [LM.alloc(8 * 128 * 2, BF16) for _ in range(NST)]; t_stg = [LM.trk() for _ in range(NST)]
        stg_i = [0]

        LF = Lay()
        hnT = LF.alloc(8 * TB * 2, BF16); t_hnT = [LF.trk() for _ in range(4)]
        xs2 = [LF.alloc(D * 2, BF16) for _ in range(2)]; t_xs2 = [LF.trk() for _ in range(2)]
        junkF = LF.alloc(D * 2, BF16); t_junkF = LF.trk()
        dtmp = LF.alloc(D * 4, F32); t_dtmp = LF.trk()
        dtmp2 = [LF.alloc(D * 4, F32) for _ in range(2)]; t_dtmp2 = [LF.trk() for _ in range(2)]
        wout_s = LF.alloc(8 * 1024 * 2, BF16); t_wout = LF.trk()
        ubv = [LF.alloc(516 * 4, F32) for _ in range(2)]; t_ubv = [LF.trk() for _ in range(2)]
        ubg = [LF.alloc(516 * 4, F32) for _ in range(2)]; t_ubg = [LF.trk() for _ in range(2)]
        cv = [LF.alloc(TB * 4, F32) for _ in range(2)]; t_cv = [LF.trk() for _ in range(2)]
        cg = [LF.alloc(TB * 4, F32) for _ in range(2)]; t_cg = [LF.trk() for _ in range(2)]
        sgb = [LF.alloc(TB * 4, F32) for _ in range(2)]; t_sg = [LF.trk() for _ in range(2)]
        actb = [LF.alloc(6 * TB * 2, BF16) for _ in range(2)]; t_act = [LF.trk() for _ in range(2)]
        wd_s = [LF.alloc(6 * 1024 * 2, BF16) for _ in range(2)]; t_wd = [LF.trk() for _ in range(2)]
        NSU = 3
        stu = [LF.alloc(8 * 256 * 2, BF16) for _ in range(NSU)]; t_stu = [LF.trk() for _ in range(NSU)]
        stu_i = [0]
        otile = [LF.alloc(D * 4, F32) for _ in range(2)]; t_ot = [LF.trk() for _ in range(2)]

        LS = Lay()
        wst = LS.alloc(8 * 1024 * 2, BF16); t_wst = LS.trk()
        cTs = LS.alloc(8 * nseq * 4, F32); t_cTs = LS.trk()
        cact = LS.alloc(8 * nseq * 2, BF16); t_cact = LS.trk()
        crep = LS.alloc(8 * nseq * 128 * 2, BF16); t_crep = LS.trk()
        bcada = LS.alloc(2048 * 4, F32); t_bcada = LS.trk()
        gtmp = LS.alloc(2048 * 4, F32); t_gtmp = LS.trk()
        stmp = LS.alloc(64 * 4, F32); t_stmp = LS.trk()

        psb = [es.enter_context(nc.psum_tensor("ps%d" % i, [128, 512], F32)) for i in range(4)]
        t_ps = [Trk() for _ in range(4)]
        psp = [es.enter_context(nc.psum_tensor("pp%d" % i, [128, 1024], F32)) for i in range(2)]
        t_pp = [[Trk(), Trk()] for _ in range(2)]
        rot = [0]

        def nxt():
            i = rot[0]
            rot[0] = (i + 1) % 4
            return psb[i], t_ps[i]

        def pvc(name, c, w=1, parts=128):
            o = pvo[name] + c
            return pvec[0:parts, o:o + w]

        def bcc(name, c=0, w=1):
            o = bco[name] + c
            return bcv[:, o:o + w]

        def v3(ap, a):
            return ap.rearrange("p (a b) -> p a b", a=a)

        def mm(out, lhsT, rhs, start, stop, reads, writes, chk=True, sig=False):
            if start and chk:
                for t in writes:
                    assert not (t.w and t.w[0][0] == "pe" and not t.r), "PSUM bank rewritten before it was read"
            R.op("pe", lambda e, o=out, l=lhsT, r=rhs, s=start, t=stop: e.matmul(o, l, r, start=s, stop=t),
                 reads=reads, writes=writes, signal=(stop or sig))

        def act(out, in_, func, reads, writes, bias=None, scale=None, accum=None):
            kw = {}
            if bias is not None:
                kw["bias"] = bias
            if scale is not None:
                kw["scale"] = scale
            if accum is not None:
                kw["accum_out"] = accum
            R.op("act", lambda e, o=out, i=in_, f=func, k=kw: e.activation(o, i, f, **k), reads=reads, writes=writes)

        def ts(eng, out, in0, s1, s2, op0, op1, reads, writes, accum=None):
            if op1 is None:
                R.op(eng, lambda e, o=out, i=in0, a=s1, p=op0: e.tensor_scalar(o, i, a, None, p),
                     reads=reads, writes=writes)
            elif accum is None:
                R.op(eng, lambda e, o=out, i=in0, a=s1, b=s2, p=op0, q=op1: e.tensor_scalar(o, i, a, b, p, q),
                     reads=reads, writes=writes)
            else:
                R.op(eng, lambda e, o=out, i=in0, a=s1, b=s2, p=op0, q=op1, c=accum:
                     e.tensor_scalar(o, i, a, b, p, q, accum_out=c), reads=reads, writes=writes)

        def tt(eng, out, in0, in1, op, reads, writes):
            R.op(eng, lambda e, o=out, a=in0, b=in1, p=op: e.tensor_tensor(o, a, b, p), reads=reads, writes=writes)

        def stt(out, in0, scalar, in1, op0, op1, reads, writes):
            R.op("dve", lambda e, o=out, a=in0, s=scalar, b=in1, p=op0, q=op1:
                 e.scalar_tensor_tensor(o, a, s, b, p, q), reads=reads, writes=writes)

        def cp(eng, out, in_, reads, writes):
            R.op(eng, lambda e, o=out, i=in_: e.tensor_copy(o, i), reads=reads, writes=writes)

        def mset(eng, ap, val, writes):
            R.op(eng, lambda e, a=ap, v=val: e.memset(a, v), writes=writes)

        def dma(q, out, in_, reads, writes, semtrk=None):
            R.dma(q, lambda e, o=out, i=in_: e.dma_start(out=o, in_=i), reads=reads, writes=writes, semtrk=semtrk)

        def cast_rows(dst, src, nrows, trk):
            r0 = 0
            while r0 < nrows:
                n = min(1024, nrows - r0)
                dma("pool", dst.ap()[r0:r0 + n, :], src.ap()[r0:r0 + n, :], [], [trk])
                r0 += n

        cast_rows(wb_in, win_d, NWG * 128, t_wb["in"])
        cast_rows(wb_oa, woa_d, 512, t_wb["oa"])
        cast_rows(wb_or, wor_d, 1024, t_wb["or"])
        cast_rows(wb_out, wout_d, 1024, t_wb["out"])
        cast_rows(wb_up, wup_d, NJ * 128, t_wb["up"])
        cast_rows(wb_down, wdown_d, DFF, t_wb["down"])
        dma("pool", wgate[:, :], wgate_d.ap(), [], [t_wgate])

        dma("sp", pvec[:, :], pvec_d.ap(), [], [t_pvec])
        dma("sp", bcv[:, :], bcv_d.ap(), [], [t_bcv])
        dma("sp", tbl[:, :], tbl_d.ap(), [], [t_tbl])
        dma("sp", cTs, cT_d.ap(), [], [t_cTs])
        dma("sp", bcada, bcada_d.ap(), [], [t_bcada])
        cp("dve", identb[:, :], tbl[:, 2048 + 128:2048 + 256], [t_tbl], [t_identb])
        mset("pool", ones64[:, :], 1.0, [t_ones64])
        mset("pool", onesf[:, :], 1.0, [t_onesf])
        mset("pool", small[:, :], 0.0, [])
        ts("dve", bq8[:, :], pvc("bq", 0, 8, 64), 0.125, None, ALU.mult, None, [t_pvec], [t_bq8])
        act(stmp[:, 0:8], pvc("lam", 0, 8), AF.Exp, [t_pvec], [t_stmp], scale=-1.0)
        act(stmp[:, 8:16], stmp[:, 0:8], AF.Ln, [t_stmp], [t_stmp], bias=1.0, scale=1.0)
        ts("dve", cneg[:, 0:8], stmp[:, 8:16], -8.0, None, ALU.mult, None, [t_stmp], [t_cneg])
        ts("dve", cneg[:, 8:16], stmp[:, 8:16], -16.0, None, ALU.mult, None, [t_stmp], [t_cneg])
        act(cact, cTs, AF.Silu, [t_cTs], [t_cact])
        cact3 = v3(cact, 8)
        crep4 = crep.rearrange("p (k b r) -> p k b r", k=8, b=nseq)
        for b in range(nseq):
            for kc in range(8):
                cp("pool", crep4[:, kc, b, :], cact3[:, kc, b:b + 1].to_broadcast([128, 128]), [t_cact], [t_crep])
        modT4 = modT[:, :].rearrange("p (k c b) -> p k c b", k=4, c=8)
        gs4 = gs[:, :].rearrange("p (k c b) -> p k c b", k=2, c=8)
        kinds = {0: ("sh1", 0), 1: ("sc1", 1), 3: ("sh2", 2), 4: ("sc2", 3)}
        wada_v = wada_d.ap().rearrange("(kc p) n -> p kc n", p=128)
        wst3 = v3(wst, 8)
        for cb in range(6):
            dma("pool", wst3, wada_v[:, :, cb * 1024:(cb + 1) * 1024], [], [t_wst])
            if cb in kinds:
                nm, ki = kinds[cb]
                ps, tp = nxt()
                for fc in range(8):
                    for kc in range(8):
                        mm(ps[:, fc * nseq:(fc + 1) * nseq], wst3[:, kc, fc * 128:(fc + 1) * 128], cact3[:, kc, :],
                           kc == 0, kc == 7, [t_wst, t_cact], [tp], chk=False)
                bap = pvc("ada_" + nm, 0, 8)
                for b in range(nseq):
                    tt("dve", modT4[:, ki, :, b], ps[:, 0:8 * nseq].rearrange("p (c b) -> p c b", c=8)[:, :, b], bap,
                       ALU.add, [tp, t_pvec], [t_modT])
            else:
                gi = 0 if cb == 2 else 1
                for b in range(nseq):
                    for half in range(2):
                        ps, tp = nxt()
                        for kc in range(8):
                            mm(ps[:, :], crep4[:, kc, b, :], wst3[:, kc, half * 512:(half + 1) * 512],
                               kc == 0, kc == 7, [t_wst, t_crep], [tp])
                        c0 = gi * 1024 + half * 512
                        tt("dve", gtmp[:, c0:c0 + 512], ps[:, :], bcada[:, c0:c0 + 512], ALU.add,
                           [tp, t_bcada], [t_gtmp])
                        dma("sp", gabc_d.ap()[b * 128:(b + 1) * 128, c0:c0 + 512], gtmp[:, c0:c0 + 512],
                            [t_gtmp], [t_gabc_d[b]], semtrk=t_gtmp)
        for wi_, (gname, ki) in enumerate((("gmix", 1), ("gffn", 3))):
            for b in range(nseq):
                ts("dve", gs4[:, wi_, :, b], modT4[:, ki, :, b], 1.0, None, ALU.add, None, [t_modT], [t_gs])
                tt("dve", gs4[:, wi_, :, b], gs4[:, wi_, :, b], pvc(gname, 0, 8), ALU.mult, [t_gs, t_pvec], [t_gs])
        R.fence(LS.trks, LM.trks)

        xnT3 = v3(xnT, 8)
        hnT3 = v3(hnT, 8)
        qT3 = v3(qT, 8)
        qiT3 = v3(qiT, 8)
        kT3 = v3(kT[:, :], 2)
        vaug4 = vaug[:, :].rearrange("p (j k c) -> p j k c", j=16, k=2)
        wis3 = v3(wis, 4)
        maskT3 = v3(maskT, 16)
        attnT3 = v3(attnT, 8)
        rnnT3 = v3(rnnT, 8)
        mergedT3 = v3(mergedT[:, :], 8)
        T01 = v3(tbl[:, 0:2048], 8)
        negmask = tbl[:, 2048:2048 + 128]
        hxr3 = v3(halo_xr[:, :], 8)
        hup3 = v3(halo_up[:, :], 44)
        wgate3 = v3(wgate[:, :], 16)
        wbin_v = wb_in.ap().rearrange("(g p) n -> g p n", p=128)
        wbup_v = wb_up.ap().rearrange("(g p) n -> g p n", p=128)
        wboa_v = wb_oa.ap().rearrange("(g p) n -> g p n", p=64)
        wbor_v = wb_or.ap().rearrange("(g p) n -> g p n", p=128)
        wbdown_v = wb_down.ap().rearrange("(j p) n -> p j n", p=128)
        wbout_v = wb_out.ap().rearrange("(kc p) n -> p kc n", p=128)
        WI_SCALE = float(64 ** -0.5 * 8 ** -0.5)

        def stage_in(name):
            i = stg_i[0]
            stg_i[0] = (i + 1) % NST
            dma("sp", stg[i], wbin_v[WIN_IDX[name]], [t_wb["in"]], [t_stg[i]])
            return v3(stg[i], 8), t_stg[i]

        def stage_raw(src_ap, parts=128):
            i = stg_i[0]
            stg_i[0] = (i + 1) % NST
            dma("sp", stg[i][0:parts, :], src_ap, [t_wb["oa"], t_wb["or"]], [t_stg[i]])
            return v3(stg[i][0:parts, :], 8), t_stg[i]

        def rms_scale(xt, t_x, junk, t_junk, xsb, t_xsb):
            ss, t_ss = smc("ss", 0)
            rs, t_rs = smc("rs", 1)
            act(junk, xt, AF.Square, [t_x], [t_junk, t_ss], accum=ss)
            ts("dve", rs, ss, 1.0 / D, EPS, ALU.mult, ALU.add, [t_ss], [t_rs])
            act(rs, rs, AF.Sqrt, [t_rs], [t_rs])
            R.op("dve", lambda e, o=rs: e.reciprocal(o, o), reads=[t_rs], writes=[t_rs])
            ts("dve", xsb, xt, rs, None, ALU.mult, None, [t_x, t_rs], [t_xsb])

        def norm_T(xt, t_x, junk, t_junk, xsl, t_xsl, which, b, dst3, t_dst, i):
            k = i % 2
            rms_scale(xt, t_x, junk, t_junk, xsl[k], t_xsl[k])
            ps, tp = nxt()
            psb16 = ps.bitcast(BF16)
            for c in range(8):
                R.op("pe", lambda e, o=psb16[:, c * 128:(c + 1) * 128], a=xsl[k][:, c * 128:(c + 1) * 128]:
                     e.transpose(o, a, identb[:, :]), reads=[t_xsl[k], t_identb], writes=[tp], signal=(c == 7))
            shk = 0 if which == 0 else 2
            for c in range(8):
                o = dst3[:, c, 128 * i:128 * i + 128]
                sc_ap = gs4[:, which, c, b:b + 1]
                sh_ap = modT4[:, shk, c, b:b + 1]
                if c % 2 == 0:
                    act(o, psb16[:, c * 128:(c + 1) * 128], AF.Identity, [tp, t_gs, t_modT], [t_dst], bias=sh_ap, scale=sc_ap)
                else:
                    ts("dve", o, psb16[:, c * 128:(c + 1) * 128], sc_ap, sh_ap, ALU.mult, ALU.add,
                       [tp, t_gs, t_modT], [t_dst])

        out_events = []

        for b in range(nseq):
            dma("sp", gabc[:, :], gabc_d.ap()[b * 128:(b + 1) * 128, :], t_gabc_d, [t_gabc])
            mset("pool", halo_xr[:, :], 0.0, t_hxr)
            mset("pool", halo_up[:, :], 0.0, t_hup)
            mset("pool", hl[:, :], 0.0, t_hl)
            mset("pool", kmaxsq[:, :], 0.0, [t_kmax])
            mset("pool", vaug[:, :], 1.0, [t_vaug])
            for g in range(nblk):
                t0 = TB * g
                for i in range(4):
                    dma("sp", xh[i][:, :], x_d.ap()[b, t0 + 128 * i:t0 + 128 * i + 128, :], [], [t_xh[i]])
                    norm_T(xh[i][:, :], t_xh[i], junkA, t_junkA, xs, t_xs, 0, b, xnT3, t_xnT[i], i)
                qmx, t_qmx = smc("qmx", 8, 8)
                kmb, t_kmb = smc("kmb", 16, 2)

                def proj_head(st3, tst, col0, dst, t_dst, bias_ap, scale, sq_dst=None):
                    ps, tp = nxt()
                    for kc in range(8):
                        mm(ps[0:64, :], st3[:, kc, col0:col0 + 64], xnT3[:, kc, :], kc == 0, kc == 7, [tst] + t_xnT, [tp])
                    act(dst, ps[0:64, :], AF.Identity, [tp, t_pvec, t_bq8], [t_dst], bias=bias_ap, scale=scale)
                    if sq_dst is not None:
                        k = sq_dst[2] % 2
                        act(sqt[k], ps[0:64, :], AF.Square, [tp, t_pvec, t_bq8], [t_sqt[k]], bias=bias_ap, scale=scale)
                        p2, tp2 = nxt()
                        mm(p2[:, :], ones64[:, :], sqt[k], True, True, [t_ones64, t_sqt[k]], [tp2])
                        R.op("dve", lambda e, o=sq_dst[0], a=p2[:, :]: e.reduce_max(o, a, AX.X),
                             reads=[tp2], writes=[sq_dst[1]])

                for cgi in range(4):
                    st3, tst = stage_in("q%d" % cgi)
                    for hh_ in range(2):
                        h = 2 * cgi + hh_
                        proj_head(st3, tst, 64 * hh_, qT3[:, h, :], t_q[h], bq8[:, h:h + 1], 0.125,
                                  (qmx[:, h:h + 1], t_qmx, h))
                st3, tst = stage_in("k")
                for kv in range(2):
                    proj_head(st3, tst, 64 * kv, kT3[:, kv, t0:t0 + TB], t_kT, pvc("bk", kv, 1, 64), 1.0,
                              (kmb[:, kv:kv + 1], t_kmb, kv))
                tt("dve", kmaxsq[:, :], kmaxsq[:, :], kmb, ALU.max, [t_kmb, t_kmax], [t_kmax])
                st3, tst = stage_in("v")
                for i in range(4):
                    ps, tp = nxt()
                    for kc in range(8):
                        mm(ps[:, 0:128], xnT3[:, kc, 128 * i:128 * i + 128], st3[:, kc, :], kc == 0, kc == 7,
                           [tst, t_xnT[i]], [tp])
                    tt("dve", vaug4[:, 4 * g + i, :, 0:64], v3(ps[:, 0:128], 2), v3(bcc("bv", 0, 128), 2), ALU.add,
                       [tp, t_bcv], [t_vaug])
                for cgi in range(4):
                    st3, tst = stage_in("qi%d" % cgi)
                    for hh_ in range(2):
                        h = 2 * cgi + hh_
                        proj_head(st3, tst, 64 * hh_, qiT3[:, h, :], t_qi[h], pvc("bqi", h, 1, 64), 1.0)
                st3, tst = stage_in("kiwi")
                proj_head(st3, tst, 0, kiT[:, t0:t0 + TB], t_kiT, pvc("bki", 0, 1, 64), 1.0)
                for i in range(4):
                    ps, tp = nxt()
                    for kc in range(8):
                        mm(ps[:, 0:8], xnT3[:, kc, 128 * i:128 * i + 128], st3[:, kc, 64:72], kc == 0, kc == 7,
                           [tst, t_xnT[i]], [tp])
                    tt("dve", wis3[:, i, :], ps[:, 0:8], bcc("bwi", 0, 8), ALU.add, [tp, t_bcv], [t_wis[i]])
                    ts("dve", wis3[:, i, :], wis3[:, i, :], WI_SCALE, None, ALU.mult, None, [t_wis[i]], [t_wis[i]])

                def rnn_front(n):
                    k = n % 2
                    st3, tst = stage_in("xr%d" % n)
                    ps, tp = nxt()
                    for kc in range(8):
                        mm(ps[:, :], st3[:, kc, :], xnT3[:, kc, :], kc == 0, kc == 7, [tst] + t_xnT, [tp])
                    cp("pool", xrb[k][:, 0:3], hxr3[:, n, :], [t_hxr[n]], [t_xrb[k]])
                    act(xrb[k][:, 3:515], ps[:, :], AF.Identity, [tp, t_pvec], [t_xrb[k]], bias=pvc("bxr", n), scale=1.0)
                    yield
                    ts("dve", xc[k], xrb[k][:, 0:512], pvc("crw", 4 * n), pvc("crb", n), ALU.mult, ALU.add,
                       [t_xrb[k], t_pvec], [t_xc[k]])
                    for kk in range(1, 4):
                        stt(xc[k], xrb[k][:, kk:kk + 512], pvc("crw", 4 * n + kk), xc[k], ALU.mult, ALU.add,
                            [t_xrb[k], t_pvec, t_xc[k]], [t_xc[k]])
                        yield
                    cp("pool", hxr3[:, n, :], xrb[k][:, 512:515], [t_xrb[k]], [t_hxr[n]])
                    cp("pool", xcb[k], xc[k], [t_xc[k]], [t_xcb[k]])
                    yield
                    st3, tst = stage_in("yr%d" % n)
                    yp, typ = nxt()
                    for kc in range(8):
                        mm(yp[:, :], st3[:, kc, :], xnT3[:, kc, :], kc == 0, kc == 7, [tst] + t_xnT, [typ])
                    act(yy[k], yp[:, :], AF.Identity, [typ, t_pvec], [t_yy[k]], bias=pvc("byr", n), scale=1.0)
                    yield

                    def back():
                        rp, trp = nxt()
                        mm(rp[:, :], wgate3[:, n, :], xcb[k], True, True, [t_wgate, t_xcb[k]], [trp])
                        ip, tip = nxt()
                        mm(ip[:, :], wgate3[:, 8 + n, :], xcb[k], True, True, [t_wgate, t_xcb[k]], [tip])
                        act(rr, rp[:, :], AF.Sigmoid, [trp, t_pvec], [t_rr], bias=pvc("brg", n), scale=1.0)
                        act(ii, ip[:, :], AF.Sigmoid, [tip, t_pvec], [t_ii], bias=pvc("big", n), scale=1.0)
                        yield
                        tt("pool", y2, yy[k], yy[k], ALU.mult, [t_yy[k]], [t_y2])
                        act(aa, rr, AF.Exp, [t_rr, t_cneg], [t_aa], scale=cneg[:, n:n + 1])
                        act(a2, rr, AF.Exp, [t_rr, t_cneg], [t_a2], scale=cneg[:, 8 + n:9 + n])
                        tt("dve", uu, xc[k], ii, ALU.mult, [t_xc[k], t_ii], [t_uu])
                        yield
                        act(a2, a2, AF.Sqrt, [t_a2], [t_a2], bias=1.0, scale=-1.0)
                        ts("pool", y2, y2, 0.044715, 1.0, ALU.mult, ALU.add, [t_y2], [t_y2])
                        tt("pool", y2, y2, yy[k], ALU.mult, [t_y2, t_yy[k]], [t_y2])
                        yield
                        tt("dve", uu, uu, a2, ALU.mult, [t_uu, t_a2], [t_uu])
                        act(y2, y2, AF.Sigmoid, [t_y2], [t_y2], scale=1.5957691216057308)
                        R.op("dve", lambda e, o=hh, a=aa, u=uu, i0=hl[:, n:n + 1]:
                             e.tensor_tensor_scan(o, a, u, i0, ALU.mult, ALU.add), reads=[t_aa, t_uu, t_hl[n]], writes=[t_hh])
                        yield
                        tt("pool", y2, y2, yy[k], ALU.mult, [t_y2, t_yy[k]], [t_y2])
                        cp("pool", hl[:, n:n + 1], hh[:, 511:512], [t_hh], [t_hl[n]])
                        tt("pool", rnnT3[:, n, :], hh, y2, ALU.mult, [t_hh, t_y2], [t_rnn[n]])
                        yield
                    return back

                def rnn_gen():
                    R.fence(merge_alias_trks, rnn_alias_trks)
                    prev = None
                    for n in range(8):
                        bk = yield from rnn_front(n)
                        if prev is not None:
                            yield from prev()
                        prev = bk
                    yield from prev()

                rgen = rnn_gen()
                rnn_done = [False]

                def pump(k=1):
                    for _ in range(k):
                        if rnn_done[0]:
                            return
                        try:
                            next(rgen)
                        except StopIteration:
                            rnn_done[0] = True

                n_points = sum(((128 * (4 * g + i + 1) + 511) // 512) * 8 for i in range(4)) + \
                    NIT * sum(1 for i in range(4) if 4 * g + i >= 2)
                RNN_YIELDS = 8 * 14
                ppp = -(-RNN_YIELDS // n_points)

                for i in range(4):
                    ti = 4 * g + i
                    nk = 128 * (ti + 1)
                    for kb in range(0, nk, 512):
                        w = min(512, nk - kb)
                        for h in range(8):
                            ps, tp = nxt()
                            mm(ps[:, 0:w], qiT3[:, h, 128 * i:128 * i + 128], kiT[:, kb:kb + w], True, True,
                               [t_qi[h], t_kiT], [tp])
                            k = h % 2
                            act(rl[k][:, 0:w], ps[:, 0:w], AF.Relu, [tp], [t_rl[k]])
                            if h == 0:
                                ts("dve", sc[:, kb:kb + w], rl[k][:, 0:w], wis3[:, i, 0:1], None, ALU.mult, None,
                                   [t_rl[k], t_wis[i]], [t_sc])
                            else:
                                stt(sc[:, kb:kb + w], rl[k][:, 0:w], wis3[:, i, h:h + 1], sc[:, kb:kb + w],
                                    ALU.mult, ALU.add, [t_rl[k], t_wis[i], t_sc], [t_sc])
                            pump(ppp)
                    if ti >= 2:
                        Rr, t_R = smc("R", 20)
                        lo, t_lo = smc("lo", 21)
                        mid, t_mid = smc("mid", 22)
                        cnt, t_cnt = smc("cnt", 23)
                        dd, t_dd = smc("dd", 24)
                        R.op("dve", lambda e, o=Rr, a=sc[:, 0:nk]: e.tensor_reduce(o, a, AX.X, ALU.max, apply_absolute_value=True),
                             reads=[t_sc], writes=[t_R])
                        tt("pool", sc[:, nk - 128:nk], sc[:, nk - 128:nk], negmask, ALU.add, [t_sc, t_tbl], [t_sc])
                        ts("dve", lo, Rr, -1.0, None, ALU.mult, None, [t_R], [t_lo])
                        for it in range(1, NIT + 1):
                            stp = float(2.0 ** (1 - it))
                            if it % 2 == 1:
                                stt(mid, Rr, stp, lo, ALU.mult, ALU.add, [t_R, t_lo], [t_mid])
                                ts("dve", junkD[:, 0:nk], sc[:, 0:nk], mid, None, ALU.is_ge, ALU.add, [t_sc, t_mid],
                                   [t_junkD, t_cnt], accum=cnt)
                                pump(ppp)
                                ts("dve", dd, cnt, NSEL - 0.5, stp, ALU.is_ge, ALU.mult, [t_cnt], [t_dd])
                            else:
                                stt(mid, Rr, -stp, lo, ALU.mult, ALU.subtract, [t_R, t_lo], [t_mid])
                                act(junkS[:, 0:nk], sc[:, 0:nk], AF.Sign, [t_sc, t_mid], [t_junkS, t_cnt], bias=mid, scale=1.0,
                                    accum=cnt)
                                pump(ppp)
                                ts("dve", dd, cnt, 2.0 * (NSEL - 0.5) - nk, stp, ALU.is_ge, ALU.mult, [t_cnt], [t_dd])
                            stt(lo, dd, Rr, lo, ALU.mult, ALU.add, [t_dd, t_R, t_lo], [t_lo])
                        ts("dve", mask[:, 0:nk], sc[:, 0:nk], lo, None, ALU.is_ge, None, [t_sc, t_lo], [t_mask])
                    else:
                        mset("pool", mask[:, 0:nk], 1.0, [t_mask])
                    j = 0
                    while j <= ti:
                        n = min(4, ti + 1 - j)
                        ps, tp = nxt()
                        psb16 = ps.bitcast(BF16)
                        for jj in range(n):
                            R.op("pe", lambda e, o=psb16[:, jj * 128:(jj + 1) * 128], a=mask[:, 128 * (j + jj):128 * (j + jj + 1)]:
                                 e.transpose(o, a, identb[:, :]), reads=[t_mask, t_identb], writes=[tp], signal=(jj == n - 1))
                        act(maskT3[:, j:j + n, 128 * i:128 * i + 128], v3(psb16[:, 0:n * 128], n), AF.Copy,
                            [tp], [t_maskT[i]])
                        j += n
                while not rnn_done[0]:
                    pump(1)

                nsb = 4 * g + 4
                prod, t_prod = smc("prod", 26, 8)
                negM, t_negM = smc("negM", 34, 8)
                negMf, t_negMf = smc("negMf", 42, 8)
                ts("dve", prod[:, 0:4], qmx[:, 0:4], kmaxsq[:, 0:1], None, ALU.mult, None, [t_qmx, t_kmax], [t_prod])
                ts("dve", prod[:, 4:8], qmx[:, 4:8], kmaxsq[:, 1:2], None, ALU.mult, None, [t_qmx, t_kmax], [t_prod])
                act(prod, prod, AF.Sqrt, [t_prod], [t_prod])
                ts("dve", negM, prod, -1.05, None, ALU.mult, None, [t_prod], [t_negM])
                tt("dve", negMf, negM, bcc("b31", 0, 8), ALU.add, [t_negM, t_bcv], [t_negMf])
                LA = 2
                steps = [(h, j) for h in range(8) for j in range(nsb)]

                def att_front(sidx):
                    h, j = steps[sidx]
                    kv = h // 4
                    d = j - 4 * g
                    c0 = 128 * max(d, 0)
                    lg, tlg = nxt()
                    mm(lg[:, c0:TB], kT3[:, kv, 128 * j:128 * j + 128], qT3[:, h, c0:TB], True, True,
                       [t_kT, t_q[h]], [tlg])
                    pt = PT[sidx % NPT]
                    tpt = t_PT[sidx % NPT]
                    n0 = max(d, 0)
                    n1 = min(d + 1, 3)
                    if d >= -1 and n0 <= n1:
                        ca, cb_ = 128 * n0, 128 * (n1 + 1)
                        ta = 128 * (n0 - d)
                        k = sidx % 3
                        tt("dve", nr[k][:, 0:cb_ - ca], lg[:, ca:cb_], T01[:, h, ta:ta + (cb_ - ca)], ALU.add,
                           [tlg, t_tbl], [t_nr[k]])
                        act(pt[:, ca:cb_], nr[k][:, 0:cb_ - ca], AF.Exp, [t_nr[k], t_negM], [tpt],
                            bias=negM[:, h:h + 1], scale=1.0)
                        fc0 = cb_
                    else:
                        fc0 = 0
                    if fc0 < TB:
                        act(pt[:, fc0:TB], lg[:, fc0:TB], AF.Exp, [tlg, t_negMf], [tpt],
                            bias=negMf[:, h:h + 1], scale=1.0)
                    tt("pool" if sidx % 2 == 0 else "dve", pt[:, c0:TB], pt[:, c0:TB], maskT3[:, j, c0:TB], ALU.mult,
                       [tpt] + t_maskT, [tpt])

                def att_back(sidx):
                    h, j = steps[sidx]
                    kv = h // 4
                    d = j - 4 * g
                    c0 = 128 * max(d, 0)
                    acc = psp[h % 2]
                    tacc = t_pp[h % 2][0]
                    pt = PT[sidx % NPT]
                    tpt = t_PT[sidx % NPT]
                    mm(acc[0:65, c0:TB], vaug4[:, j, kv, :], pt[:, c0:TB], j == 0, j == nsb - 1,
                       [t_vaug, tpt], [tacc], sig=True)
                    if j == nsb - 1:
                        k = h % 2
                        act(osb[k], acc[0:65, 0:TB], AF.Copy, [tacc], [t_osb[k]])
                        R.op("dve", lambda e, o=osb[k][64:65, :]: e.reciprocal(o, o), reads=[t_osb[k]], writes=[t_osb[k]])
                        bc_, tbc = nxt()
                        mm(bc_[0:64, :], onesf[64:65, 0:64], osb[k][64:65, :], True, True, [t_onesf, t_osb[k]], [tbc])
                        tt("dve", attnT3[:, h, :], osb[k][0:64, :], bc_[0:64, :], ALU.mult, [t_osb[k], tbc], [t_attn[h]])

                for sidx in range(len(steps) + LA):
                    if sidx < len(steps):
                        att_front(sidx)
                    if sidx >= LA:
                        att_back(sidx - LA)

                R.fence(rnn_alias_trks, merge_alias_trks)
                for m in range(8):
                    A, tA = psp[m % 2][:, 0:512], t_pp[m % 2][0]
                    Rp, tRp = psp[m % 2][:, 512:1024], t_pp[m % 2][1]
                    st3, tst = stage_raw(wboa_v[m], 64)
                    for h in range(8):
                        mm(A, st3[:, h, :], attnT3[:, h, :], h == 0, h == 7, [tst, t_attn[h]], [tA])
                    st3, tst = stage_raw(wbor_v[m])
                    for kc in range(8):
                        mm(Rp, st3[:, kc, :], rnnT3[:, kc, :], kc == 0, kc == 7, [tst, t_rnn[kc]], [tRp])
                    st3, tst = stage_in("ga%d" % m)
                    gp, tgp = nxt()
                    for kc in range(8):
                        mm(gp[:, :], st3[:, kc, :], xnT3[:, kc, :], kc == 0, kc == 7, [tst] + t_xnT, [tgp])
                    act(sa, gp[:, :], AF.Sigmoid, [tgp, t_pvec], [t_sa], bias=pvc("bga", m), scale=1.0)
                    st3, tst = stage_in("gb%d" % m)
                    gp, tgp = nxt()
                    for kc in range(8):
                        mm(gp[:, :], st3[:, kc, :], xnT3[:, kc, :], kc == 0, kc == 7, [tst] + t_xnT, [tgp])
                    act(sbg, gp[:, :], AF.Sigmoid, [tgp, t_pvec], [t_sbg], bias=pvc("bgb", m), scale=1.0)
                    tt("dve", t1, A, sa, ALU.mult, [tA, t_sa], [t_t1])
                    tt("dve", t2, Rp, sbg, ALU.mult, [tRp, t_sbg], [t_t2])
                    tt("pool", mergedT3[:, m, :], t1, t2, ALU.add, [t_t1, t_t2], [t_merged[m]])

                R.fence(LM.trks, LF.trks)
                dma("sp", v3(wout_s, 8), wbout_v, [t_wb["out"]], [t_wout])
                wout3 = v3(wout_s, 8)
                for i in range(4):
                    pp, tpp = psp[i % 2], t_pp[i % 2]
                    for half in range(2):
                        for m in range(8):
                            mm(pp[:, half * 512:(half + 1) * 512], mergedT3[:, m, 128 * i:128 * i + 128],
                               wout3[:, m, half * 512:(half + 1) * 512], m == 0, m == 7, [t_merged[m], t_wout], [tpp[half]])
                    tt("dve", dtmp, pp[:, :], gabc[:, 0:1024], ALU.mult, tpp + [t_gabc], [t_dtmp])
                    tt("pool", xh[i][:, :], dtmp, xh[i][:, :], ALU.add, [t_dtmp, t_xh[i]], [t_xh[i]])
                    norm_T(xh[i][:, :], t_xh[i], junkF, t_junkF, xs2, t_xs2, 1, b, hnT3, t_hnT[i], i)
                chunks = [(gi, jj, j) for gi, js in enumerate(FF_GROUPS) for jj, j in enumerate(js)]

                def ffn_front(c):
                    gi, jj, j = chunks[c]
                    k2 = gi % 2
                    if jj == 0:
                        js = FF_GROUPS[gi]
                        dma("sp", v3(wd_s[k2], 6)[:, 0:len(js), :], wbdown_v[:, js[0]:js[0] + len(js), :],
                            [t_wb["down"]], [t_wd[k2]])
                    si = stu_i[0]
                    stu_i[0] = (si + 1) % NSU
                    dma("sp", stu[si], wbup_v[j], [t_wb["up"]], [t_stu[si]])
                    su3 = v3(stu[si], 8)
                    u = c % 2
                    for (half, ub, tub, cc, tcc, jx) in ((0, ubv[u], t_ubv[u], cv[u], t_cv[u], j),
                                                       (1, ubg[u], t_ubg[u], cg[u], t_cg[u], NJ + j)):
                        ps, tp = nxt()
                        for kc in range(8):
                            mm(ps[:, :], su3[:, kc, half * 128:(half + 1) * 128], hnT3[:, kc, :], kc == 0, kc == 7,
                               [t_stu[si]] + t_hnT, [tp])
                        cp("pool", ub[:, 0:2], hup3[:, jx, :], [t_hup[jx]], [tub])
                        act(ub[:, 2:514], ps[:, :], AF.Copy, [tp], [tub])
                        act(cc, ps[:, :], AF.Identity, [tp, t_pvec], [tcc], bias=pvc("cfb", jx), scale=pvc("cfw", 3 * jx + 2))
                        for k in range(0, 2):
                            stt(cc, ub[:, k:k + 512], pvc("cfw", 3 * jx + k), cc, ALU.mult, ALU.add,
                                [tub, t_pvec, tcc], [tcc])
                        cp("pool", hup3[:, jx, :], ub[:, 512:514], [tub], [t_hup[jx]])

                def ffn_back(c):
                    gi, jj, j = chunks[c]
                    k2 = gi % 2
                    u = c % 2
                    act(sgb[u], cg[u], AF.Silu, [t_cg[u]], [t_sg[u]])
                    tt("pool", v3(actb[k2], 6)[:, jj, :], sgb[u], cv[u], ALU.mult, [t_sg[u], t_cv[u]], [t_act[k2]])

                def ffn_down(gi):
                    js = FF_GROUPS[gi]
                    k2 = gi % 2
                    wd3 = v3(wd_s[k2], 6)
                    act3 = v3(actb[k2], 6)
                    for i in range(4):
                        pp, tpp = psp[i % 2], t_pp[i % 2]
                        for half in range(2):
                            for jj in range(len(js)):
                                mm(pp[:, half * 512:(half + 1) * 512], act3[:, jj, 128 * i:128 * i + 128],
                                   wd3[:, jj, half * 512:(half + 1) * 512], jj == 0, jj == len(js) - 1,
                                   [t_act[k2], t_wd[k2]], [tpp[half]])
                        dk = i % 2
                        tt("dve", dtmp2[dk], pp[:, :], gabc[:, 1024:2048], ALU.mult, tpp + [t_gabc], [t_dtmp2[dk]])
                        tt("pool", xh[i][:, :], dtmp2[dk], xh[i][:, :], ALU.add, [t_dtmp2[dk], t_xh[i]], [t_xh[i]])

                pending_down = []
                for c in range(len(chunks) + 1):
                    if c < len(chunks):
                        ffn_front(c)
                    if c >= 1:
                        ffn_back(c - 1)
                        for (gi_, due) in list(pending_down):
                            if c >= due:
                                ffn_down(gi_)
                                pending_down.remove((gi_, due))
                        gi_p, jj_p, _ = chunks[c - 1]
                        if jj_p == len(FF_GROUPS[gi_p]) - 1:
                            pending_down.append((gi_p, c + 1))
                for (gi_, due) in pending_down:
                    ffn_down(gi_)
                for i in range(4):
                    ss, t_ss = smc("ss", 0)
                    rs, t_rs = smc("rs", 1)
                    act(junkF, xh[i][:, :], AF.Square, [t_xh[i]], [t_junkF, t_ss], accum=ss)
                    ts("dve", rs, ss, 1.0 / D, EPS, ALU.mult, ALU.add, [t_ss], [t_rs])
                    act(rs, rs, AF.Sqrt, [t_rs], [t_rs])
                    R.op("dve", lambda e, o=rs: e.reciprocal(o, o), reads=[t_rs], writes=[t_rs])
                    k = i % 2
                    stt(otile[k], xh[i][:, :], rs, bcc("gfin", 0, 1024), ALU.mult, ALU.mult, [t_xh[i], t_rs, t_bcv], [t_ot[k]])
                    dma("sp", out_d.ap()[b, t0 + 128 * i:t0 + 128 * i + 128, :], otile[k], [t_ot[k]], [])
                R.fence(LF.trks, LM.trks)

        R.wait_all("sp", t_ot)
        R.wait_all("sp", t_xh)

        assert not any(R.pending.values()), R.pending
        def replay(name, e):
            for it in R.streams[name]:
                if it[0] == "w":
                    e.wait_ge(sems[it[1]], it[2])
                elif it[0] == "i":
                    it[1](e).then_inc(sems[name], 1)
                elif it[0] == "n":
                    it[1](e)
                else:
                    it[1](e).then_inc(sems[it[2]], 16)

        with nc.Block() as block:
            @block.tensor
            def _(e):
                replay("pe", e)

            @block.scalar
            def _(e):
                replay("act", e)

            @block.vector
            def _(e):
                replay("dve", e)

            @block.gpsimd
            def _(e):
                replay("pool", e)

            @block.sync
            def _(e):
                replay("sp", e)
    return nc


def _PVN():
    pvo, _ = _offsets()
    return max(pvo.values()) + 32


def make_in_maps(inputs, nseq=4, ncores=NCORES):
    inp = {k: np.asarray(v, np.float32) for k, v in inputs.items()}
    sh, _, _ = _layout_shared(inp)
    assert sh["pvec"].shape[1] <= _PVN()
    pv = np.zeros((128, _PVN()), np.float32)
    pv[:, :sh["pvec"].shape[1]] = sh["pvec"]
    sh["pvec"] = pv
    maps = []
    for c in range(ncores):
        xs_ = np.ascontiguousarray(inp["x"][c * nseq:(c + 1) * nseq])
        cc = inp["c"][c * nseq:(c + 1) * nseq]
        cT = np.ascontiguousarray(cc.reshape(nseq, 8, 128).transpose(2, 1, 0)).reshape(128, 8 * nseq)
        m = dict(sh)
        m["x"] = xs_
        m["cT"] = np.ascontiguousarray(cT)
        maps.append(m)
    return maps


_NC_CACHE = {}


def kernel(**inputs):
    nseq = 4
    if nseq not in _NC_CACHE:
        _NC_CACHE[nseq] = build_nc(nseq)
    nc = _NC_CACHE[nseq]
    maps = make_in_maps(inputs, nseq, NCORES)
    res = run_bass_kernel_spmd(nc, maps, core_ids=list(range(NCORES)))
    out = np.concatenate([np.asarray(r["out"], np.float32).reshape(nseq, S, D) for r in res.results], axis=0)
    return out
```
